# Optimizing a Trainium2 kernel written in Bass

```python
import jax, jax.numpy as jnp
from jax import lax
import numpy as np

D_MODEL = 2048
BATCH = 2
SEQ = 8192
DEPTH = 1

N_META = 16
CONV_CH = D_MODEL
CONV_WIDTH = 31
GLA_HEADS = 4
GLA_DK = D_MODEL // 2 // GLA_HEADS
GLA_DV = D_MODEL // GLA_HEADS
GLA_GATE_RANK = 16
GLA_TAU = 16.0
GLA_CHUNK = 64
PEER_HEADS = 8
PEER_NKEYS = 128
PEER_N = PEER_NKEYS * PEER_NKEYS
PEER_DK = 256
PEER_TOPK = 16
PEER_TOKEN_BLOCK = 128
EPS = 1e-6

COL_CONV = 2 * CONV_CH
COL_Q = GLA_HEADS * GLA_DK
COL_K = GLA_HEADS * GLA_DK
COL_V = GLA_HEADS * GLA_DV
COL_R = GLA_HEADS * GLA_DV
COL_A = GLA_GATE_RANK
COL_MERGE = 2 * D_MODEL
COL_TOTAL = COL_CONV + COL_Q + COL_K + COL_V + COL_R + COL_A + COL_MERGE

kernel_name = "hybrid_conv_gla_peer_block"


def _rmsnorm(x, g):
    xf = x.astype(jnp.float32)
    y = xf * lax.rsqrt(jnp.mean(xf * xf, axis=-1, keepdims=True) + EPS)
    return (y * g.astype(jnp.float32)).astype(x.dtype)


def _conv_module(a, conv_w, conv_b, ln_g, ln_b, w_out):
    h = a[..., :CONV_CH] * jax.nn.sigmoid(a[..., CONV_CH:])
    hp = jnp.pad(h, ((0, 0), (CONV_WIDTH - 1, 0), (0, 0)))
    h = lax.conv_general_dilated(hp, conv_w[:, None, :], window_strides=(1,), padding='VALID',
                                 dimension_numbers=('NWC', 'WIO', 'NWC'),
                                 feature_group_count=CONV_CH) + conv_b
    hf = h.astype(jnp.float32)
    mu = jnp.mean(hf, axis=-1, keepdims=True)
    var = jnp.mean(jnp.square(hf - mu), axis=-1, keepdims=True)
    hf = (hf - mu) * lax.rsqrt(var + EPS) * ln_g.astype(jnp.float32) + ln_b.astype(jnp.float32)
    h = jax.nn.silu(hf).astype(a.dtype)
    return h @ w_out


def _gla_branch(q, k, v, r, a_low, w_a2, b_a, norm_g, w_o):
    B, L, _ = q.shape
    H, dk, dv, C = GLA_HEADS, GLA_DK, GLA_DV, GLA_CHUNK
    pad = C - N_META
    log_a = jax.nn.log_sigmoid((a_low @ w_a2 + b_a).astype(jnp.float32)) / GLA_TAU

    def to_chunks(t, d):
        t = jnp.pad(t.astype(jnp.float32), ((0, 0), (pad, 0), (0, 0)))
        n = t.shape[1] // C
        return t.reshape(B, n, C, H, d).transpose(1, 0, 3, 2, 4)

    qc = to_chunks(q, dk) * (dk ** -0.5)
    kc = to_chunks(k, dk)
    vc = to_chunks(v, dv)
    gc = to_chunks(log_a, dk)
    mask = jnp.tril(jnp.ones((C, C), dtype=bool))[None, None, :, :, None]

    def step(S, inp):
        qb, kb, vb, gb = inp
        b = jnp.cumsum(gb, axis=2)
        o_inter = jnp.einsum('bhcd,bhde->bhce', qb * jnp.exp(b), S)
        diff = b[:, :, :, None, :] - b[:, :, None, :, :]
        decay = jnp.exp(jnp.where(mask, diff, -jnp.inf))
        att = jnp.einsum('bhid,bhjd,bhijd->bhij', qb, kb, decay)
        o_intra = jnp.einsum('bhij,bhje->bhie', att, vb)
        b_last = b[:, :, -1:, :]
        S_new = jnp.exp(b_last[:, :, 0, :])[..., None] * S + \
            jnp.einsum('bhjd,bhje->bhde', kb * jnp.exp(b_last - b), vb)
        return S_new, o_inter + o_intra

    S0 = jnp.zeros((B, H, dk, dv), jnp.float32)
    _, o = lax.scan(step, S0, (qc, kc, vc, gc))
    n = o.shape[0]
    o = o.transpose(1, 0, 3, 2, 4).reshape(B, n * C, H, dv)[:, pad:]
    o = o * lax.rsqrt(jnp.mean(o * o, axis=-1, keepdims=True) + EPS)
    o = o * norm_g.astype(jnp.float32).reshape(H, dv)
    o = o * jax.nn.silu(r.astype(jnp.float32)).reshape(B, L, H, dv)
    return o.reshape(B, L, H * dv).astype(w_o.dtype) @ w_o


def _peer(xn, wq, k1, k2, U, V):
    B, L, D = xn.shape
    PH, NK, K, half = PEER_HEADS, PEER_NKEYS, PEER_TOPK, PEER_DK // 2
    q = (xn @ wq).astype(jnp.float32).reshape(B, L, PH, PEER_DK)
    s1 = jnp.einsum('blhd,hnd->blhn', q[..., :half], k1.astype(jnp.float32))
    s2 = jnp.einsum('blhd,hnd->blhn', q[..., half:], k2.astype(jnp.float32))
    v1, i1 = lax.top_k(s1, K)
    v2, i2 = lax.top_k(s2, K)
    cand = (v1[..., :, None] + v2[..., None, :]).reshape(B, L, PH, K * K)
    cidx = (i1[..., :, None] * NK + i2[..., None, :]).reshape(B, L, PH, K * K)
    top_s, pos = lax.top_k(cand, K)
    idx = jnp.take_along_axis(cidx, pos, axis=-1)
    gate = jax.nn.softmax(top_s, axis=-1)

    T = B * L
    TB = PEER_TOKEN_BLOCK
    nblk = -(-T // TB)
    padT = nblk * TB - T
    xt = jnp.pad(xn.reshape(T, D), ((0, padT), (0, 0))).reshape(nblk, TB, D)
    it = jnp.pad(idx.reshape(T, PH, K), ((0, padT), (0, 0), (0, 0))).reshape(nblk, TB, PH, K)
    gt = jnp.pad(gate.reshape(T, PH, K), ((0, padT), (0, 0), (0, 0))).reshape(nblk, TB, PH, K)

    def block(args):
        xb, ib, gb = args
        u = U[ib]
        h = jax.nn.gelu(jnp.einsum('thkd,td->thk', u, xb).astype(jnp.float32), approximate=False)
        w = (gb * h).astype(xb.dtype)
        return jnp.einsum('thk,thkd->td', w, V[ib])

    y = lax.map(block, (xt, it, gt)).reshape(nblk * TB, D)[:T]
    return y.reshape(B, L, D).astype(xn.dtype)


def setup_inputs(seed: int = 0) -> dict:
    key = jax.random.key(seed)
    ks = jax.random.split(key, 24)

    def nrm(k, shape, scale):
        return jax.random.normal(k, shape, jnp.float32) * scale

    Dp = DEPTH
    return {
        "x": nrm(ks[0], (BATCH, SEQ, D_MODEL), 1.0),
        "meta_tokens": nrm(ks[1], (N_META, D_MODEL), 1.0),
        "norm1_g": 1.0 + nrm(ks[2], (Dp, D_MODEL), 0.02),
        "w_in": nrm(ks[3], (Dp, D_MODEL, COL_TOTAL), D_MODEL ** -0.5),
        "w_alpha2": nrm(ks[4], (Dp, GLA_GATE_RANK, GLA_HEADS * GLA_DK), GLA_GATE_RANK ** -0.5),
        "b_alpha": nrm(ks[5], (Dp, GLA_HEADS * GLA_DK), 0.1),
        "conv_w": nrm(ks[6], (Dp, CONV_WIDTH, CONV_CH), CONV_WIDTH ** -0.5),
        "conv_b": nrm(ks[7], (Dp, CONV_CH), 0.02),
        "conv_ln_g": 1.0 + nrm(ks[8], (Dp, CONV_CH), 0.02),
        "conv_ln_b": nrm(ks[9], (Dp, CONV_CH), 0.02),
        "w_conv_out": nrm(ks[10], (Dp, CONV_CH, D_MODEL), CONV_CH ** -0.5),
        "gla_norm_g": 1.0 + nrm(ks[11], (Dp, GLA_HEADS * GLA_DV), 0.02),
        "w_gla_out": nrm(ks[12], (Dp, GLA_HEADS * GLA_DV, D_MODEL), (GLA_HEADS * GLA_DV) ** -0.5),
        "w_out": nrm(ks[13], (Dp, D_MODEL, D_MODEL), D_MODEL ** -0.5),
        "norm2_g": 1.0 + nrm(ks[14], (Dp, D_MODEL), 0.02),
        "peer_wq": nrm(ks[15], (Dp, D_MODEL, PEER_HEADS * PEER_DK), D_MODEL ** -0.5),
        "peer_k1": nrm(ks[16], (Dp, PEER_HEADS, PEER_NKEYS, PEER_DK // 2), (PEER_DK // 2) ** -0.5),
        "peer_k2": nrm(ks[17], (Dp, PEER_HEADS, PEER_NKEYS, PEER_DK // 2), (PEER_DK // 2) ** -0.5),
        "peer_u": nrm(ks[18], (Dp, PEER_N, D_MODEL), D_MODEL ** -0.5),
        "peer_v": nrm(ks[19], (Dp, PEER_N, D_MODEL), PEER_HEADS ** -0.5),
        "normf_g": 1.0 + nrm(ks[20], (D_MODEL,), 0.02),
    }


def reference(x, meta_tokens, norm1_g, w_in, w_alpha2, b_alpha, conv_w, conv_b, conv_ln_g, conv_ln_b,
              w_conv_out, gla_norm_g, w_gla_out, w_out, norm2_g, peer_wq, peer_k1, peer_k2,
              peer_u, peer_v, normf_g):
    B = x.shape[0]
    meta = jnp.broadcast_to(meta_tokens[None].astype(x.dtype), (B, N_META, D_MODEL))
    h = jnp.concatenate([meta, x], axis=1)
    offs = [COL_CONV, COL_Q, COL_K, COL_V, COL_R, COL_A]
    cuts = [int(c) for c in np.cumsum(offs)]
    for l in range(DEPTH):
        xn = _rmsnorm(h, norm1_g[l])
        proj = xn @ w_in[l]
        a_conv, q, k, v, r, a_low, g_merge = jnp.split(proj, cuts, axis=-1)
        y_conv = _conv_module(a_conv, conv_w[l], conv_b[l], conv_ln_g[l], conv_ln_b[l], w_conv_out[l])
        y_gla = _gla_branch(q, k, v, r, a_low, w_alpha2[l], b_alpha[l], gla_norm_g[l], w_gla_out[l])
        gates = jax.nn.sigmoid(g_merge.astype(jnp.float32))
        mixed = gates[..., :D_MODEL] * y_conv.astype(jnp.float32) + gates[..., D_MODEL:] * y_gla.astype(jnp.float32)
        h = h + mixed.astype(h.dtype) @ w_out[l]
        h = h + _peer(_rmsnorm(h, norm2_g[l]), peer_wq[l], peer_k1[l], peer_k2[l], peer_u[l], peer_v[l])
    return _rmsnorm(h, normf_g)[:, N_META:]
```

```python
import numpy as np
from contextlib import ExitStack
import concourse.bass as bass
import concourse.mybir as mybir
from concourse.bass_utils import run_bass_kernel_spmd

F32 = mybir.dt.float32
BF16 = mybir.dt.bfloat16
AF = mybir.ActivationFunctionType
ALU = mybir.AluOpType
AX = mybir.AxisListType

D = 2048
NCH = 16
TT = 512
Q0, K0, V0, R0, A0, M0, M1, COLT = 4096, 5120, 6144, 8192, 10240, 10256, 12304, 14352
EPS = 1e-6
NEG = -1.0e30


class Sched:
    COMPUTE = ("pe", "act", "dve", "pool")
    ENGS = ("pe", "act", "dve", "pool", "sp")

    def __init__(self, nc):
        self.nc = nc
        self.ins = {e: [] for e in self.ENGS}
        self.wr = {}
        self.rd = {}
        self.seen = {e: {} for e in self.ENGS}
        self.dma_cnt = {}
        self.signal = {e: set() for e in self.COMPUTE}

    def _need(self, eng, deps):
        waits = []
        seen = self.seen[eng]
        for s, c in deps.items():
            if seen.get(s, 0) >= c:
                continue
            seen[s] = c
            waits.append((s, c))
            if s in self.COMPUTE:
                self.signal[s].add(c)
        return waits

    def op(self, eng, fn, reads=(), writes=(), dma=None):
        idx = len(self.ins[eng]) + 1
        if dma is not None:
            cnt = self.dma_cnt.get(dma, 0) + 1
            self.dma_cnt[dma] = cnt
            stream, count = ("dma", dma), cnt
        else:
            stream, count = eng, idx
        deps = {}

        def add(s, c):
            if deps.get(s, 0) < c:
                deps[s] = c
        rk = [k for b in reads for k in b.keys]
        wk = [k for b in writes for k in b.keys]
        for k in rk:
            for s, c in self.wr.get(k, {}).items():
                if s == stream and eng == "pe" and dma is None:
                    continue
                add(s, c)
        for k in wk:
            for s, c in self.rd.get(k, {}).items():
                if s == stream:
                    continue
                add(s, c)
            for s, c in self.wr.get(k, {}).items():
                if s == stream:
                    continue
                add(s, c)
        waits = self._need(eng, deps)
        for k in rk:
            d = self.rd.setdefault(k, {})
            if d.get(stream, 0) < count:
                d[stream] = count
        for k in wk:
            self.wr[k] = {stream: count}
            self.rd[k] = {}
        self.ins[eng].append(dict(fn=fn, waits=waits, dma=dma, idx=idx))

    def final_wait_all(self, eng="sp"):
        deps = {("dma", k): c for k, c in self.dma_cnt.items()}
        waits = self._need(eng, deps)
        self.ins[eng].append(dict(fn=None, waits=waits, dma=None, idx=len(self.ins[eng]) + 1))

    def emit(self, es):
        nc = self.nc
        sems = {}
        for e in self.COMPUTE:
            sems[e] = es.enter_context(nc.semaphore("s_" + e))
        for k in self.dma_cnt:
            sems[("dma", k)] = es.enter_context(nc.semaphore("d_" + str(k)))
        rank = {}
        for e in self.COMPUTE:
            rank[e] = {c: n + 1 for n, c in enumerate(sorted(self.signal[e]))}
        block = es.enter_context(nc.Block())
        engobj = {"pe": "tensor", "act": "scalar", "dve": "vector", "pool": "gpsimd", "sp": "sync"}

        def make(e):
            def body(eng):
                for rec in self.ins[e]:
                    for s, c in rec["waits"]:
                        if s in self.COMPUTE:
                            eng.wait_ge(sems[s], rank[s][c])
                        else:
                            eng.wait_ge(sems[s], 16 * c)
                    if rec["fn"] is None:
                        continue
                    inst = rec["fn"](eng)
                    if rec["dma"] is not None:
                        inst.then_inc(sems[("dma", rec["dma"])], 16)
                    elif rec["idx"] in self.signal[e]:
                        inst.then_inc(sems[e], 1)
            return body
        for e in self.ENGS:
            if self.ins[e]:
                getattr(block, engobj[e])(make(e))


class Buf:
    def __init__(self, base_ap, name, off, words, gran=128):
        self.base = base_ap
        self.off = off
        self.words = words
        self.name = name
        self.keys = [(name, s) for s in range(off // gran, (off + words + gran - 1) // gran)]

    def f(self, o=0, n=None):
        n = self.words - o if n is None else n
        assert o + n <= self.words
        return self.base[:, self.off + o:self.off + o + n]

    def b(self, o=0, n=None):
        n = self.words * 2 - o if n is None else n
        assert o + n <= self.words * 2
        return self.base[:, self.off:self.off + self.words].bitcast(BF16)[:, o:o + n]

    def sub(self, o, n):
        return Buf(self.base, self.name, self.off + o, n)


def build(NPRE, NMAIN, NEG_=32, dbg=None):
    nc = bass.Bass("TRN2", target_bir_lowering=False)
    NT = NPRE + NMAIN
    ROWS = NT * TT
    NE = NEG_ * 512

    def din(name, shape):
        return nc.dram_tensor(name, shape, F32, kind="ExternalInput").ap()
    xin = din("xin", [ROWS, D])
    w_in = din("w_in", [D, COLT])
    wa2e_d = din("wa2e", [17, 1024])
    convw_d = din("convw", [128, 16 * 31])
    vecs_d = din("vecs", [128, 6 * 16])
    gfb_d = din("gfb", [128, D])
    g1b_d = din("g1b", [128, D])
    g2b_d = din("g2b", [128, D])
    cst_d = din("cst", [128, 4 * 128])
    w_co = din("w_co", [D, D])
    w_go = din("w_go", [D, D])
    w_o = din("w_o", [D, D])
    w_q = din("w_q", [D, D])
    k12_d = din("k12", [128, 2 * 8 * 128])
    UT = din("UT", [D, NE])
    Vd = din("Vd", [NE, D])
    out = nc.dram_tensor("out", [NMAIN * TT, D], F32, kind="ExternalOutput").ap()
    dbg_out = None
    if dbg:
        dbg_out = nc.dram_tensor("dbg", [128, dbg], F32, kind="ExternalOutput").ap()

    es = ExitStack()
    with es:
        S = Sched(nc)

        def sbt(name, words):
            t = es.enter_context(nc.sbuf_tensor("sb_" + name, [128, words], F32))
            return Buf(t[:], name, 0, words)

        def pst(name):
            t = es.enter_context(nc.psum_tensor(name, [128, 512], F32))
            return Buf(t[:], name, 0, 512, gran=512)

        xnT = sbt("xnT", 4096)
        WR = [sbt("wr%d" % i, 4096) for i in range(4)]
        S32 = sbt("S32", 4096)
        halo = sbt("halo", 512)
        cst = sbt("cst", 512)
        vecs = sbt("vecs", 96)
        convw = sbt("convw", 496)
        wa2e = sbt("wa2e", 1024)
        wa16 = sbt("wa16", 128)
        misc = sbt("misc", 1024)
        identb = sbt("identb", 64)
        A = sbt("A", 8192)
        B = sbt("B", 8192)
        C = sbt("C", 8192)
        P = [pst("P%d" % i) for i in range(8)]

        ident = cst.sub(0, 128)
        triinc = cst.sub(128, 128)
        trirev = cst.sub(256, 128)
        masku = cst.sub(384, 128)
        ones_b = sbt("ones", 128)

        xnT3 = xnT.b().rearrange("p (k t) -> p k t", k=16)
        XN = [(xnT, xnT3), (C.sub(0, 4096), C.sub(0, 4096).b().rearrange("p (k t) -> p k t", k=16))]

        def vcol(i, k):
            return vecs.f(i * 16 + k, 1)
        VN1, VN2, VCB, VLG, VLB, VGG = range(6)


        accn = [0]

        def acc():
            accn[0] += 1
            return P[accn[0] % 4]

        def DMA(eng, out_ap, in_ap, key, reads=(), writes=()):
            S.op(eng, lambda e: e.dma_start(out=out_ap, in_=in_ap), reads=reads, writes=writes, dma=key)

        def OP(eng, fn, reads, writes):
            S.op(eng, fn, reads=reads, writes=writes)

        DMA("sp", cst.f(), cst_d, "cst", writes=[cst])
        DMA("sp", vecs.f(), vecs_d, "vecs", writes=[vecs])
        DMA("sp", convw.f(), convw_d, "convw", writes=[convw])
        DMA("sp", wa2e.f()[0:17, :], wa2e_d, "wa2e", writes=[wa2e])
        DMA("pool", wa16.b().rearrange("p (k c) -> p k c", k=16),
            w_in.rearrange("(k p) n -> p k n", p=128)[:, :, A0:A0 + 16], "wa16", writes=[wa16])
        OP("dve", lambda e: e.memset(S32.f(), 0.0), [], [S32])
        OP("dve", lambda e: e.memset(halo.f(), 0.0), [], [halo])
        OP("dve", lambda e: e.memset(ones_b.f(), 1.0), [], [ones_b])
        OP("dve", lambda e: e.tensor_copy(out=identb.b(), in_=ident.f()), [ident], [identb])

        steps = []

        def wgroup(mat, c0, n=512):
            return mat.rearrange("(k p) n -> p k n", p=128)[:, :, c0:c0 + n]

        def run_steps():
            LOOK = 2
            nload = [0]

            def issue(i):
                loads, _ = steps[i]
                buf = WR[i % 4]
                for (c_off, ncol, src) in loads:
                    if src is None:
                        continue
                    dst = buf.b().rearrange("p (k c) -> p k c", k=16)[:, :, c_off:c_off + ncol] if src[0] == "w" else \
                        buf.b().rearrange("p (j d) -> p j d", j=4)
                    DMA("pool", dst, src[1], "wr%d" % (i % 4), writes=[buf])
            for i in range(min(LOOK, len(steps))):
                issue(i)
            for i in range(len(steps)):
                steps[i][1](WR[i % 4])
                if i + LOOK < len(steps):
                    issue(i + LOOK)

        def step(loads, fn):
            steps.append((loads, fn))

        def w3(buf):
            return buf.b().rearrange("p (k c) -> p k c", k=16)

        def norm_batch(srcs, gb, xs2, junk, xn, groups=((0, 1, 2, 3),), loader=None):
            xb, x3 = xn
            ss4 = misc.sub(768, 4)
            t4 = misc.sub(896, 4)
            r4 = misc.sub(0, 4)
            for grp in groups:
                g0, gn = grp[0], len(grp)
                for ts in grp:
                    if loader is not None:
                        loader(ts)
                    OP("act", lambda e, ts=ts: e.activation(out=junk.b(), in_=srcs[ts].f(), func=AF.Square,
                                                            accum_out=ss4.f(ts, 1)), [srcs[ts]], [junk, ss4])
                OP("dve", lambda e, g0=g0, gn=gn: e.tensor_scalar(out=t4.f(g0, gn), in0=ss4.f(g0, gn), scalar1=1.0 / D,
                                                                  scalar2=EPS, op0=ALU.mult, op1=ALU.add), [ss4], [t4])
                OP("act", lambda e, g0=g0, gn=gn: e.activation(out=t4.f(g0, gn), in_=t4.f(g0, gn), func=AF.Sqrt),
                   [t4], [t4])
                OP("dve", lambda e, g0=g0, gn=gn: e.reciprocal(out=r4.f(g0, gn), in_=t4.f(g0, gn)), [t4], [r4])
                for ts in grp:
                    xs = xs2[ts % 2]
                    OP("dve", lambda e, ts=ts, xs=xs: e.scalar_tensor_tensor(
                        out=xs.b(), in0=srcs[ts].f(), scalar=r4.f(ts, 1), in1=gb.f(), op0=ALU.mult, op1=ALU.mult),
                        [srcs[ts], r4, gb], [xs])
                    for half in range(2):
                        pt = P[4 + half]

                        def fnt(e, half=half, pt=pt, xs=xs):
                            last = None
                            for k8 in range(8):
                                k = half * 8 + k8
                                last = e.transpose(out=pt.b()[:, k8 * 128:(k8 + 1) * 128],
                                                   in_=xs.b()[:, k * 128:(k + 1) * 128], identity=identb.b())
                            return last
                        OP("pe", fnt, [xs, identb], [pt])
                        OP("act", lambda e, half=half, pt=pt, ts=ts: e.copy(
                            out=x3[:, half * 8:half * 8 + 8, ts * 128:(ts + 1) * 128],
                            in_=pt.b().rearrange("p (k t) -> p k t", k=8)), [pt], [xb])

        def proj_fm(wbuf, c0, dst_ps, xn=None):
            xb, x3 = xn if xn is not None else (xnT, xnT3)

            def fn(e):
                last = None
                for k in range(16):
                    last = e.matmul(dst_ps.f(), lhsT=w3(wbuf)[:, k, c0:c0 + 128], rhs=x3[:, k, :],
                                    start=(k == 0), stop=(k == 15))
                return last
            OP("pe", fn, [wbuf, xb], [dst_ps])

        def proj_tm(wbuf, c0, n, ts, dst_ps, xn=None):
            xb, x3 = xn if xn is not None else (xnT, xnT3)

            def fn(e):
                last = None
                for k in range(16):
                    last = e.matmul(dst_ps.f(0, n), lhsT=x3[:, k, ts * 128:(ts + 1) * 128],
                                    rhs=w3(wbuf)[:, k, c0:c0 + n], start=(k == 0), stop=(k == 15))
                return last
            OP("pe", fn, [wbuf, xb], [dst_ps])

        def tile_prog(ti):
            main = ti >= NPRE
            lastpre = ti == NPRE - 1
            row0 = ti * TT
            XNt = XN[0] if main else XN[ti % 2]

            def emit_ph1(tj):
                mainj = tj >= NPRE
                rowj = tj * TT
                xnj = XN[0] if mainj else XN[tj % 2]

                def ph1(_w):
                    if mainj:
                        srcs = [A.sub(ts * 2048, 2048) for ts in range(4)]
                        gb = C.sub(0, 2048)
                        xs2_ = [B.sub(2048, 1024), B.sub(3072, 1024)]
                        junk_ = B.sub(4096, 1024)
                        groups = ((0, 1, 2, 3),)
                        keys = ["xst0", "xst1", "xst2", "xst3"]
                    else:
                        stage = [C.sub(4096, 2048), C.sub(6144, 2048)]
                        srcs = [stage[ts % 2] for ts in range(4)]
                        gb = A.sub(1024, 2048)
                        xs2_ = [A.sub(3072, 1024), A.sub(4096, 1024)]
                        junk_ = A.sub(5120, 1024)
                        groups = ((0, 1), (2, 3))
                        keys = ["xst0", "xst1", "xst0", "xst1"]
                    DMA("sp", gb.f(), g1b_d, "gb", writes=[gb])

                    def loader(ts):
                        DMA("sp", srcs[ts].f(), xin[rowj + ts * 128: rowj + (ts + 1) * 128, :], keys[ts],
                            writes=[srcs[ts]])
                    norm_batch(srcs, gb, xs2_, junk_, xnj, groups=groups, loader=loader)
                step([], ph1)

            if ti == 0 or main:
                emit_ph1(ti)

            cacc = A
            cacc3 = A.f().rearrange("p (k t) -> p k t", k=16)
            sconv = C.sub(0, 4096)
            sconv3 = sconv.b().rearrange("p (k t) -> p k t", k=16)
            mixT = C.sub(4096, 4096)
            mixT3 = mixT.b().rearrange("p (k t) -> p k t", k=16)
            ofT = sconv
            ofT3 = sconv3

            if main or lastpre:
                dgb = [B.sub(0, 2048), B.sub(2048, 2048)]
                hTb = [B.sub(4096, 384), B.sub(4480, 384)]
                sigt = B.sub(4864, 512)
                sqt = B.sub(5376, 512)
                S1b, S2b = P[6], P[7]
                for cg in range(4):
                    holder = {}

                    def keep(wa, holder=holder):
                        holder["wa"] = wa
                    step([(0, 512, ("w", wgroup(w_in, cg * 512)))], keep)

                    def conv_g(wg, cg=cg, holder=holder):
                        wa = holder["wa"]
                        for cb in range(4):
                            blk = cg * 4 + cb
                            hb = hTb[blk % 2]
                            dg = dgb[blk % 2]
                            pa, pg = acc(), acc()
                            proj_fm(wa, cb * 128, pa, XNt)
                            proj_fm(wg, cb * 128, pg, XNt)
                            OP("act", lambda e, pg=pg: e.activation(out=sigt.f(), in_=pg.f(), func=AF.Sigmoid),
                               [pg], [sigt])
                            OP("dve", lambda e, hb=hb, blk=blk: e.tensor_copy(
                                out=hb.b(0, 32), in_=halo.b(blk * 32, 32)), [halo], [hb])
                            OP("dve", lambda e, hb=hb, pa=pa: e.tensor_tensor(
                                out=hb.b(32, 512), in0=pa.f(), in1=sigt.f(), op=ALU.mult), [pa, sigt], [hb])
                            OP("dve", lambda e, hb=hb, blk=blk: e.tensor_copy(
                                out=halo.b(blk * 32, 32), in_=hb.b(512, 32)), [hb], [halo])
                            if not main:
                                continue
                            cblk = cacc.sub(blk * 512, 512)
                            for w in range(31):
                                OP("dve", lambda e, dg=dg, blk=blk, w=w: e.tensor_scalar(
                                    out=dg.b(w * 128, 128), in0=ident.f(), scalar1=convw.f(blk * 31 + w, 1),
                                    scalar2=None, op0=ALU.mult), [ident, convw], [dg.sub(w * 64, 64)])
                            pc = acc()

                            def fnc(e, pc=pc, dg=dg, hb=hb):
                                last = None
                                for w in range(31):
                                    last = e.matmul(pc.f(), lhsT=dg.b(w * 128, 128), rhs=hb.b(w + 2, 512),
                                                    start=(w == 0), stop=(w == 30))
                                return last
                            OP("pe", fnc, [dg, hb], [pc])
                            OP("act", lambda e, cblk=cblk, pc=pc, blk=blk: e.activation(
                                out=cblk.f(), in_=pc.f(), func=AF.Identity, bias=vcol(VCB, blk)), [pc, vecs], [cblk])
                            OP("act", lambda e, pc=pc, blk=blk: e.activation(
                                out=sqt.f(), in_=pc.f(), func=AF.Square, bias=vcol(VCB, blk)), [pc, vecs], [sqt])
                            OP("pe", lambda e, cblk=cblk, blk=blk: e.matmul(
                                S1b.f(), lhsT=ones_b.f(), rhs=cblk.f(), start=(blk == 0), stop=(blk == 15)),
                               [ones_b, cblk], [S1b])
                            OP("pe", lambda e, blk=blk: e.matmul(
                                S2b.f(), lhsT=ones_b.f(), rhs=sqt.f(), start=(blk == 0), stop=(blk == 15)),
                               [ones_b, sqt], [S2b])
                    step([(0, 512, ("w", wgroup(w_in, 2048 + cg * 512)))], conv_g)

            if main:
                mu = B.sub(6144, 512)
                Ar = B.sub(6656, 512)
                Bm = B.sub(7168, 512)
                zt = B.sub(7680, 512)
                gt = B.sub(6144, 512)

                def ln_apply(_w):
                    S1b, S2b = P[6], P[7]
                    OP("dve", lambda e: e.tensor_scalar(out=mu.f(), in0=S1b.f(), scalar1=1.0 / D, scalar2=None,
                                                        op0=ALU.mult), [S1b], [mu])
                    OP("dve", lambda e: e.tensor_tensor(out=zt.f(), in0=mu.f(), in1=mu.f(), op=ALU.mult), [mu], [zt])
                    OP("dve", lambda e: e.scalar_tensor_tensor(out=Ar.f(), in0=S2b.f(), scalar=1.0 / D, in1=zt.f(),
                                                               op0=ALU.mult, op1=ALU.subtract), [S2b, zt], [Ar])
                    OP("dve", lambda e: e.tensor_scalar(out=Ar.f(), in0=Ar.f(), scalar1=EPS, scalar2=None,
                                                        op0=ALU.add), [Ar], [Ar])
                    OP("act", lambda e: e.activation(out=Ar.f(), in_=Ar.f(), func=AF.Sqrt), [Ar], [Ar])
                    OP("dve", lambda e: e.reciprocal(out=Ar.f(), in_=Ar.f()), [Ar], [Ar])
                    OP("dve", lambda e: e.scalar_tensor_tensor(out=Bm.f(), in0=mu.f(), scalar=-1.0, in1=Ar.f(),
                                                               op0=ALU.mult, op1=ALU.mult), [mu, Ar], [Bm])
                    for blk in range(16):
                        cblk = cacc.sub(blk * 512, 512)
                        OP("dve", lambda e, cblk=cblk: e.tensor_tensor(out=zt.f(), in0=cblk.f(), in1=Ar.f(),
                                                                       op=ALU.mult), [cblk, Ar], [zt])
                        OP("dve", lambda e: e.tensor_tensor(out=zt.f(), in0=zt.f(), in1=Bm.f(), op=ALU.add),
                           [zt, Bm], [zt])
                        OP("act", lambda e, blk=blk: e.activation(
                            out=sconv3[:, blk, :], in_=zt.f(), func=AF.Silu, bias=vcol(VLB, blk),
                            scale=vcol(VLG, blk)), [zt, vecs], [sconv])
                step([], ln_apply)

                def outproj_gate(first, act3, actbuf, wmat, mcol0):
                    for dg in range(4):
                        holder = {}

                        def keep(w, holder=holder):
                            holder["w"] = w
                        step([(0, 512, ("w", wgroup(wmat, dg * 512)))], keep)

                        def cons(wm, dg=dg, holder=holder):
                            wo = holder["w"]
                            for db in range(4):
                                dblk = dg * 4 + db
                                py, pm = acc(), acc()

                                def fn(e, db=db, py=py):
                                    last = None
                                    for k in range(16):
                                        last = e.matmul(py.f(), lhsT=w3(wo)[:, k, db * 128:(db + 1) * 128],
                                                        rhs=act3[:, k, :], start=(k == 0), stop=(k == 15))
                                    return last
                                OP("pe", fn, [wo, actbuf], [py])
                                proj_fm(wm, db * 128, pm)
                                OP("act", lambda e, pm=pm: e.activation(out=gt.f(), in_=pm.f(), func=AF.Sigmoid),
                                   [pm], [gt])
                                if first:
                                    OP("dve", lambda e, py=py, dblk=dblk: e.tensor_tensor(
                                        out=mixT3[:, dblk, :], in0=py.f(), in1=gt.f(), op=ALU.mult),
                                        [py, gt], [mixT])
                                else:
                                    OP("dve", lambda e, py=py: e.tensor_tensor(
                                        out=zt.f(), in0=py.f(), in1=gt.f(), op=ALU.mult), [py, gt], [zt])
                                    OP("dve", lambda e, dblk=dblk: e.tensor_tensor(
                                        out=mixT3[:, dblk, :], in0=zt.f(), in1=mixT3[:, dblk, :], op=ALU.add),
                                        [zt, mixT], [mixT])
                        step([(0, 512, ("w", wgroup(w_in, mcol0 + dg * 512)))], cons)
                outproj_gate(True, sconv3, sconv, w_co, M0)

            qTh = B.sub(0, 512)
            kTh = B.sub(512, 512)
            ktok = B.sub(1024, 1024)
            vbf = B.sub(2048, 1024)
            lbuf = B.sub(3072, 1024)
            EbT = B.sub(4096, 1024)
            Erev = B.sub(5120, 1024)
            EnbT = lbuf
            aTe = A.sub(0, 512)
            khat = A.sub(512, 512)
            qtl = A.sub(1024, 512)
            ktl = A.sub(1536, 512)
            Sbf2 = [A.sub(2048, 512), A.sub(2560, 512)]
            attm = A.sub(3072, 256)
            rs4 = A.sub(3328, 512)
            ocp = A.sub(3840, 2048)
            sq4 = A.sub(5888, 2048)
            rt = sq4.sub(0, 512)
            qTh3 = qTh.b().rearrange("p (c t) -> p c t", c=2)
            kTh3 = kTh.b().rearrange("p (c t) -> p c t", c=2)
            qTh4 = qTh.b().rearrange("p (c s t) -> p s c t", c=2, s=4)
            kTh4 = kTh.b().rearrange("p (c s t) -> p s c t", c=2, s=4)
            ktok3 = ktok.f().rearrange("p (s c) -> p s c", s=4)
            vbf3 = vbf.b().rearrange("p (s c) -> p s c", s=4)
            S323 = S32.f().rearrange("p (k e) -> p k e", k=8)
            EbT4 = EbT.f().rearrange("p (s c t) -> p s c t", s=4, c=2)
            EnbT4 = EnbT.f().rearrange("p (s c t) -> p s c t", s=4, c=2)
            qtl4 = qtl.b().rearrange("p (s c t) -> p s c t", s=4, c=2)
            ktl4 = ktl.b().rearrange("p (s c t) -> p s c t", s=4, c=2)

            def gla_pre(_w):
                pa = acc()
                OP("dve", lambda e: e.memset(aTe.f()[0:32, :], 1.0), [], [aTe])

                def fn(e):
                    last = None
                    wv = wa16.b().rearrange("p (k c) -> p k c", k=16)
                    for k in range(16):
                        last = e.matmul(pa.f()[0:16, :], lhsT=wv[:, k, :], rhs=XNt[1][:, k, :],
                                        start=(k == 0), stop=(k == 15))
                    return last
                OP("pe", fn, [wa16, XNt[0]], [pa])
                OP("act", lambda e: e.copy(out=aTe.f()[0:16, :], in_=pa.f()[0:16, :]), [pa], [aTe])
            step([], gla_pre)

            for h in range(4):
                def gla_qk(wqk, h=h):
                    if main:
                        for c in range(2):
                            pq = acc()
                            proj_fm(wqk, c * 128, pq)
                            OP("act", lambda e, pq=pq, c=c: e.activation(out=qTh3[:, c, :], in_=pq.f(), func=AF.Copy,
                                                                         scale=0.0625), [pq], [qTh])
                            pk = acc()
                            proj_fm(wqk, 256 + c * 128, pk)
                            OP("act", lambda e, pk=pk, c=c: e.copy(out=kTh3[:, c, :], in_=pk.f()), [pk], [kTh])
                    PZ = [P[4], P[5]]
                    PB = [P[6], P[7]]

                    def fnz(e):
                        last = None
                        for ts in range(4):
                            last = e.matmul(PZ[ts // 2].f((ts % 2) * 256, 256),
                                            lhsT=aTe.f()[0:17, ts * 128:(ts + 1) * 128],
                                            rhs=wa2e.f()[0:17, h * 256:(h + 1) * 256], start=True, stop=True)
                        return last
                    OP("pe", fnz, [aTe, wa2e], PZ)
                    for hf in range(2):
                        OP("act", lambda e, hf=hf: e.activation(out=lbuf.f(hf * 512, 512), in_=PZ[hf].f(),
                                                                func=AF.Exp, scale=-1.0),
                           [PZ[hf]], [lbuf.sub(hf * 512, 512)])
                    OP("act", lambda e: e.activation(out=lbuf.f(), in_=lbuf.f(), func=AF.Ln, bias=1.0), [lbuf], [lbuf])

                    for ts in range(4):
                        pk = acc()
                        proj_tm(wqk, 256, 256, ts, pk, XNt)
                        OP("act", lambda e, pk=pk, ts=ts: e.copy(out=ktok3[:, ts, :], in_=pk.f(0, 256)), [pk], [ktok])
                    def fnb(e):
                        last = None
                        for ts in range(4):
                            for c in range(2):
                                last = e.matmul(PB[ts // 2].f((ts % 2) * 256 + c * 128, 128),
                                                lhsT=lbuf.f(ts * 256 + c * 128, 128), rhs=triinc.f(),
                                                start=True, stop=True)
                        return last
                    OP("pe", fnb, [lbuf, triinc], PB)

                    def fnr(e):
                        last = None
                        for ts in range(4):
                            last = e.matmul(PZ[ts // 2].f((ts % 2) * 256, 256), lhsT=trirev.f(),
                                            rhs=lbuf.f(ts * 256, 256), start=True, stop=True)
                        return last
                    OP("pe", fnr, [lbuf, trirev], PZ)
                qk_loads = [(256, 256, ("w", wgroup(w_in, K0 + h * 256, 256)))]
                if main:
                    qk_loads.insert(0, (0, 256, ("w", wgroup(w_in, Q0 + h * 256, 256))))
                step(qk_loads, gla_qk)

                def gla_v(wv, h=h):
                    def emit_vproj():
                        for ts in range(4):
                            pv = acc()
                            proj_tm(wv, 0, 512, ts, pv, XNt)
                            OP("act", lambda e, pv=pv, ts=ts: e.copy(out=vbf3[:, ts, :], in_=pv.f()), [pv], [vbf])
                    PZ = [P[4], P[5]]
                    PB = [P[6], P[7]]
                    for hf in range(2):
                        OP("act", lambda e, hf=hf: e.activation(out=EbT.f(hf * 512, 512), in_=PB[hf].f(), func=AF.Exp),
                           [PB[hf]], [EbT.sub(hf * 512, 512)])
                        OP("act", lambda e, hf=hf: e.activation(out=Erev.f(hf * 512, 512), in_=PZ[hf].f(), func=AF.Exp),
                           [PZ[hf]], [Erev.sub(hf * 512, 512)])
                    OP("dve", lambda e: e.tensor_tensor(out=khat.b(), in0=ktok.f(), in1=Erev.f(), op=ALU.mult),
                       [ktok, Erev], [khat])
                    if main:
                        for hf in range(2):
                            OP("act", lambda e, hf=hf: e.activation(out=EnbT.f(hf * 512, 512), in_=PB[hf].f(),
                                                                    func=AF.Exp, scale=-1.0),
                               [PB[hf]], [EnbT.sub(hf * 512, 512)])
                        OP("dve", lambda e: e.tensor_tensor(out=qtl4, in0=qTh4, in1=EbT4, op=ALU.mult),
                           [qTh, EbT], [qtl])
                        OP("dve", lambda e: e.tensor_tensor(out=ktl4, in0=kTh4, in1=EnbT4, op=ALU.mult),
                           [kTh, EnbT], [ktl])
                        emit_vproj()
                        pat = acc()

                        def fna(e, pat=pat):
                            last = None
                            for ts in range(4):
                                for c in range(2):
                                    last = e.matmul(pat.f(ts * 128, 128), lhsT=ktl4[:, ts, c, :], rhs=qtl4[:, ts, c, :],
                                                    start=(c == 0), stop=(c == 1))
                            return last
                        OP("pe", fna, [ktl, qtl], [pat])
                        OP("dve", lambda e, pat=pat: e.tensor_tensor(
                            out=attm.b().rearrange("p (s i) -> p s i", s=4),
                            in0=pat.f().rearrange("p (s i) -> p s i", s=4),
                            in1=masku.f().unsqueeze(1).to_broadcast([128, 4, 128]), op=ALU.mult),
                            [pat, masku], [attm])
                    if not main:
                        emit_vproj()
                    for ts in range(4):
                        pSs = []
                        for c in range(2):
                            pS = acc()
                            OP("pe", lambda e, pS=pS, c=c, ts=ts: e.matmul(
                                pS.f(), lhsT=khat.b(ts * 256 + c * 128, 128), rhs=vbf3[:, ts, :], start=True, stop=True),
                               [khat, vbf], [pS])
                            pSs.append(pS)
                        if main:
                            Sbf = Sbf2[ts % 2]
                            Sbf3 = Sbf.b().rearrange("p (c e) -> p c e", c=2)
                            OP("act", lambda e, Sbf3=Sbf3: e.copy(out=Sbf3, in_=S323[:, 2 * h:2 * h + 2, :]),
                               [S32.sub(2 * h * 512, 1024)], [Sbf])
                            po = acc()

                            def fno(e, po=po, ts=ts, Sbf3=Sbf3):
                                last = None
                                for eb in range(4):
                                    e.matmul(po.f(eb * 128, 128), lhsT=vbf3[:, ts, eb * 128:(eb + 1) * 128],
                                             rhs=attm.b(ts * 128, 128), start=True, stop=False)
                                    for c in range(2):
                                        last = e.matmul(po.f(eb * 128, 128), lhsT=Sbf3[:, c, eb * 128:(eb + 1) * 128],
                                                        rhs=qtl4[:, ts, c, :], start=False, stop=(c == 1))
                                return last
                            OP("pe", fno, [vbf, attm, Sbf, qtl], [po])
                            OP("act", lambda e, po=po, ts=ts: e.copy(out=ocp.f(ts * 512, 512), in_=po.f()),
                               [po], [ocp.sub(ts * 512, 512)])
                            OP("act", lambda e, po=po, ts=ts: e.activation(out=sq4.f(ts * 512, 512), in_=po.f(),
                                                                          func=AF.Square),
                               [po], [sq4.sub(ts * 512, 512)])
                        for c in range(2):
                            sblk = S32.sub((2 * h + c) * 512, 512)
                            OP("dve", lambda e, pS=pSs[c], c=c, ts=ts: e.scalar_tensor_tensor(
                                out=S323[:, 2 * h + c, :], in0=S323[:, 2 * h + c, :],
                                scalar=EbT.f(ts * 256 + c * 128 + 127, 1), in1=pS.f(), op0=ALU.mult, op1=ALU.add),
                               [sblk, EbT, pSs[c]], [sblk])
                    if main:
                        pss = acc()

                        def fns(e, pss=pss):
                            last = None
                            for ts in range(4):
                                for eb in range(4):
                                    last = e.matmul(pss.f(ts * 128, 128), lhsT=ones_b.f(),
                                                    rhs=sq4.f(ts * 512 + eb * 128, 128), start=(eb == 0), stop=(eb == 3))
                            return last
                        OP("pe", fns, [ones_b, sq4], [pss])
                        OP("dve", lambda e, pss=pss: e.tensor_scalar(out=rs4.f(), in0=pss.f(), scalar1=1.0 / 512,
                                                                     scalar2=EPS, op0=ALU.mult, op1=ALU.add),
                           [pss], [rs4])
                        OP("act", lambda e: e.activation(out=rs4.f(), in_=rs4.f(), func=AF.Sqrt), [rs4], [rs4])
                        OP("dve", lambda e: e.reciprocal(out=rs4.f(), in_=rs4.f()), [rs4], [rs4])
                        OP("dve", lambda e: e.tensor_tensor(
                            out=ofT3[:, 4 * h:4 * h + 4, :].rearrange("p e (s i) -> p s e i", s=4),
                            in0=ocp.f().rearrange("p (s e i) -> p s e i", s=4, e=4),
                            in1=rs4.f().rearrange("p (s i) -> p s i", s=4).unsqueeze(2).to_broadcast([128, 4, 4, 128]),
                            op=ALU.mult), [ocp, rs4], [ofT])
                step([(0, 512, ("w", wgroup(w_in, V0 + h * 512)))], gla_v)
                if (not main) and h == 1 and ti + 1 < NPRE:
                    emit_ph1(ti + 1)

                if main:
                    def gla_r(wr_, h=h):
                        for eb in range(4):
                            pr = acc()
                            proj_fm(wr_, eb * 128, pr)
                            OP("act", lambda e, pr=pr: e.activation(out=rt.f(), in_=pr.f(), func=AF.Silu), [pr], [rt])
                            OP("dve", lambda e, eb=eb: e.scalar_tensor_tensor(
                                out=ofT3[:, 4 * h + eb, :], in0=rt.f(), scalar=vcol(VGG, 4 * h + eb),
                                in1=ofT3[:, 4 * h + eb, :], op0=ALU.mult, op1=ALU.mult), [rt, vecs, ofT], [ofT])
                    step([(0, 512, ("w", wgroup(w_in, R0 + h * 512)))], gla_r)

            if not main:
                return
            zt = B.sub(7680, 512)
            gt = B.sub(6144, 512)
            outproj_gate(False, ofT3, ofT, w_go, M1)

            hres = A
            hres3 = A.f().rearrange("p (s d) -> p s d", s=4)
            for dg in range(4):
                def oproj(wo, dg=dg):
                    if dg == 0:
                        for ts in range(4):
                            DMA("sp", hres3[:, ts, :], xin[row0 + ts * 128: row0 + (ts + 1) * 128, :], "hres%d" % ts,
                                writes=[hres.sub(ts * 2048, 2048)])
                    for ts in range(4):
                        ph = acc()

                        def fn(e, ph=ph, ts=ts):
                            last = None
                            for k in range(16):
                                last = e.matmul(ph.f(), lhsT=mixT3[:, k, ts * 128:(ts + 1) * 128], rhs=w3(wo)[:, k, :],
                                                start=(k == 0), stop=(k == 15))
                            return last
                        OP("pe", fn, [mixT, wo], [ph])
                        hb = hres.sub(ts * 2048 + dg * 512, 512)
                        OP("dve", lambda e, ph=ph, hb=hb: e.tensor_tensor(out=hb.f(), in0=hb.f(), in1=ph.f(),
                                                                          op=ALU.add), [hb, ph], [hb])
                step([(0, 512, ("w", wgroup(w_o, dg * 512)))], oproj)

            s1b = B.sub(0, 4096)
            s2b = B.sub(4096, 4096)
            s14 = s1b.f().rearrange("p (s h n) -> p s h n", s=4, h=8)
            s24 = s2b.f().rearrange("p (s h n) -> p s h n", s=4, h=8)
            Dg = C.sub(0, 2048)
            Dg4 = Dg.b().rearrange("p (s h n) -> p s h n", s=4, h=8)
            k1T = C.sub(2048, 1024)
            k2T = C.sub(3072, 1024)
            qf = C.sub(4096, 2048)
            qf3 = qf.f().rearrange("p (b t) -> p b t", b=4)
            Pc = C.sub(6144, 2048)
            xs2 = C.sub(4096, 1024)
            T1 = misc.sub(16, 128)
            T2 = misc.sub(144, 128)
            PT = misc.sub(272, 128)
            m1 = misc.sub(400, 8)
            m2 = misc.sub(408, 8)
            Zs = misc.sub(416, 8)
            rZ = misc.sub(424, 8)
            kap = misc.sub(432, 32)
            m8 = misc.sub(464, 8)
            tmpr = misc.sub(512, 256)
            T13 = T1.f().rearrange("p (h k) -> p h k", h=8)
            T23 = T2.f().rearrange("p (h k) -> p h k", h=8)
            PT3 = PT.f().rearrange("p (h k) -> p h k", h=8)

            def ph5a(_w):
                gb2 = C.sub(0, 2048)
                DMA("sp", gb2.f(), g2b_d, "gb", writes=[gb2])
                norm_batch([hres.sub(ts * 2048, 2048) for ts in range(4)], gb2,
                           [C.sub(4096, 1024), C.sub(5120, 1024)], B.sub(4096, 1024), XN[0])
                DMA("sp", C.f(2048, 2048), k12_d, "k12", writes=[k1T, k2T])
            step([], ph5a)
            for hp in range(4):
                def ph5q(wq, hp=hp):
                    for blk in range(4):
                        pq = acc()
                        proj_fm(wq, blk * 128, pq)
                        OP("act", lambda e, pq=pq, blk=blk: e.copy(out=qf3[:, blk, :], in_=pq.f()), [pq], [qf])
                    for hh in range(2):
                        h = 2 * hp + hh
                        for ts in range(4):
                            p1, p2 = P[4 + (ts % 2) * 2], P[5 + (ts % 2) * 2]
                            OP("pe", lambda e, p1=p1, hh=hh, h=h, ts=ts: e.matmul(
                                p1.f(0, 128), lhsT=qf3[:, 2 * hh, ts * 128:(ts + 1) * 128], rhs=k1T.f(h * 128, 128),
                                start=True, stop=True), [qf, k1T], [p1])
                            OP("pe", lambda e, p2=p2, hh=hh, h=h, ts=ts: e.matmul(
                                p2.f(0, 128), lhsT=qf3[:, 2 * hh + 1, ts * 128:(ts + 1) * 128], rhs=k2T.f(h * 128, 128),
                                start=True, stop=True), [qf, k2T], [p2])
                            OP("act", lambda e, p1=p1, h=h, ts=ts: e.copy(out=s14[:, ts, h, :], in_=p1.f(0, 128)),
                               [p1], [s1b.sub(ts * 1024 + h * 128, 128)])
                            OP("act", lambda e, p2=p2, h=h, ts=ts: e.copy(out=s24[:, ts, h, :], in_=p2.f(0, 128)),
                               [p2], [s2b.sub(ts * 1024 + h * 128, 128)])
                step([(0, 512, ("w", wgroup(w_q, hp * 512)))], ph5q)

            ACT_HEADS = (6, 7)
            POOL_GH_HEADS = ()

            def top16(src_ap_fn, srcbuf, dst3, dstbuf, h, nsrc):
                OP("dve", lambda e: e.max(out=dst3[:, h, 0:8], in_=src_ap_fn()), [srcbuf], [dstbuf])
                OP("dve", lambda e: e.match_replace(out=tmpr.f(0, nsrc), in_to_replace=dst3[:, h, 0:8],
                                                    in_values=src_ap_fn(), imm_value=NEG), [srcbuf, dstbuf], [tmpr])
                OP("dve", lambda e: e.max(out=dst3[:, h, 8:16], in_=tmpr.f(0, nsrc)), [tmpr], [dstbuf])

            def ph5t(_w):
                for ts in range(4):
                    s1t = s1b.sub(ts * 1024, 1024)
                    s2t = s2b.sub(ts * 1024, 1024)
                    for h in range(8):
                        top16(lambda h=h, ts=ts: s14[:, ts, h, :], s1t, T13, T1, h, 128)
                        top16(lambda h=h, ts=ts: s24[:, ts, h, :], s2t, T23, T2, h, 128)
                    OP("dve", lambda e: e.tensor_copy(out=m1.f(), in_=T13[:, :, 0]), [T1], [m1])
                    OP("dve", lambda e: e.tensor_copy(out=m2.f(), in_=T23[:, :, 0]), [T2], [m2])
                    for (sb_, s4, mm, Tb, T3) in ((s1t, s14, m1, T1, T13), (s2t, s24, m2, T2, T23)):
                        OP("dve", lambda e, s4=s4, mm=mm, ts=ts: e.tensor_tensor(
                            out=s4[:, ts, :, :], in0=s4[:, ts, :, :],
                            in1=mm.f().unsqueeze(2).to_broadcast([128, 8, 128]), op=ALU.subtract), [sb_, mm], [sb_])
                        OP("act", lambda e, s4=s4, ts=ts: e.activation(out=s4[:, ts, :, :], in_=s4[:, ts, :, :],
                                                                       func=AF.Exp), [sb_], [sb_])
                        OP("dve", lambda e, T3=T3, mm=mm: e.tensor_tensor(
                            out=T3, in0=T3, in1=mm.f().unsqueeze(2).to_broadcast([128, 8, 16]), op=ALU.subtract),
                            [Tb, mm], [Tb])
                        OP("act", lambda e, T3=T3: e.activation(out=T3, in_=T3, func=AF.Exp), [Tb], [Tb])
                    Pc4 = Pc.f().rearrange("p (h a b) -> p h a b", h=8, a=16)
                    OP("dve", lambda e: e.tensor_tensor(
                        out=Pc4, in0=T13.unsqueeze(3).to_broadcast([128, 8, 16, 16]),
                        in1=T23.unsqueeze(2).to_broadcast([128, 8, 16, 16]), op=ALU.mult), [T1, T2], [Pc])
                    for h in range(8):
                        top16(lambda h=h: Pc.f(h * 256, 256), Pc, PT3, PT, h, 256)
                    OP("dve", lambda e: e.tensor_reduce(out=Zs.f(), in_=PT3, axis=AX.X, op=ALU.add), [PT], [Zs])
                    OP("dve", lambda e: e.reciprocal(out=rZ.f(), in_=Zs.f()), [Zs], [rZ])
                    OP("dve", lambda e, ts=ts: e.tensor_copy(out=kap.f(ts * 8, 8), in_=PT3[:, :, 15]), [PT], [kap])
                    for h in range(8):
                        OP("dve", lambda e, ts=ts, h=h: e.tensor_scalar(
                            out=Dg4[:, ts, h, :], in0=ident.f(), scalar1=rZ.f(h, 1), scalar2=None, op0=ALU.mult),
                            [ident, rZ], [Dg])
            step([], ph5t)

            LP = 4096
            Pb = [C.sub(LP + i * 512, 512) for i in range(3)]
            Gh = [C.sub(LP + 1536 + i * 256, 256) for i in range(2)]
            gel4 = C.sub(LP + 2048, 1024)
            gel43 = gel4.b().rearrange("p (j t) -> p j t", j=4)
            wT = C.sub(LP + 3072, 1024)
            wT3 = wT.b().rearrange("p (j t) -> p j t", j=4)
            HT = P[0]
            YB = [P[1], P[6], P[7]]
            GT = [P[2], P[3], P[4], P[5]]
            cnt = [0, 0, 0]
            pst_ = {}

            def vmm_one(wvp, ts, db):
                cnt[2] += 1
                py = YB[cnt[2] % 3]
                wv3 = wvp.b().rearrange("p (j d) -> p j d", j=4)

                def fnv(e, py=py, ts=ts, db=db):
                    last = None
                    for j in range(4):
                        last = e.matmul(py.f(), lhsT=wT3[:, j, ts * 128:(ts + 1) * 128],
                                        rhs=wv3[:, j, db * 512:(db + 1) * 512], start=(j == 0), stop=(j == 3))
                    return last
                OP("pe", fnv, [wT, wvp], [py])
                hb = hres.sub(ts * 2048 + db * 512, 512)
                OP("dve", lambda e, py=py, hb=hb: e.tensor_tensor(out=hb.f(), in0=hb.f(), in1=py.f(),
                                                                  op=ALU.add), [hb, py], [hb])

            def gbuild_pair(eg, ts, hq):
                ghs = []
                for h in (2 * hq, 2 * hq + 1):
                    cnt[0] += 1
                    cnt[1] += 1
                    pb_, gh = Pb[cnt[0] % 3], Gh[cnt[1] % 2]
                    if h in ACT_HEADS:
                        for a in range(4):
                            OP("act", lambda e, pb_=pb_, ts=ts, h=h, a=a: e.activation(
                                out=pb_.f(a * 128, 128), in_=s24[:, ts, h, :], func=AF.Copy,
                                scale=s14[:, ts, h, 4 * eg + a:4 * eg + a + 1]),
                                [s1b.sub(ts * 1024 + h * 128, 128), s2b.sub(ts * 1024 + h * 128, 128)],
                                [pb_.sub(a * 128, 128)])
                    else:
                        OP("pool", lambda e, pb_=pb_, ts=ts, h=h: e.tensor_tensor(
                            out=pb_.f().rearrange("p (a n) -> p a n", a=4),
                            in0=s14[:, ts, h, 4 * eg:4 * eg + 4].unsqueeze(2).to_broadcast([128, 4, 128]),
                            in1=s24[:, ts, h, :].unsqueeze(1).to_broadcast([128, 4, 128]), op=ALU.mult),
                            [s1b.sub(ts * 1024 + h * 128, 128), s2b.sub(ts * 1024 + h * 128, 128)], [pb_])
                    OP("pool" if h in POOL_GH_HEADS else "dve", lambda e, pb_=pb_, gh=gh, ts=ts, h=h: e.scalar_tensor_tensor(
                        out=gh.b(), in0=pb_.f(), scalar=kap.f(ts * 8 + h, 1), in1=pb_.f(),
                        op0=ALU.is_ge, op1=ALU.mult), [pb_, kap], [gh])
                    ghs.append((gh, h))
                return ghs

            def gt_mm(ghs, ts):
                for gh, h in ghs:
                    def fng(e, gh=gh, ts=ts, h=h):
                        last = None
                        for j in range(4):
                            last = e.matmul(GT[j].f(ts * 128, 128), lhsT=gh.b(j * 128, 128),
                                            rhs=Dg4[:, ts, h, :], start=(h == 0), stop=(h == 7))
                        return last
                    OP("pe", fng, [gh, Dg], GT)

            for eg in range(NEG_):
                holder = {}

                def keepu(w, holder=holder):
                    holder["u"] = w
                step([(0, 512, ("w", wgroup(UT, eg * 512)))], keepu)

                def peer(wv, eg=eg, holder=holder):
                    wu = holder["u"]
                    wvp = pst_.get("v")
                    for ts in range(4):
                        proj_fm(wu, ts * 128, HT)
                        OP("act", lambda e, ts=ts: e.activation(out=gel43[:, ts, :], in_=HT.f(), func=AF.Gelu),
                           [HT], [gel4.sub(ts * 256, 256)])
                        for hq in range(4):
                            ghs = gbuild_pair(eg, ts, hq)
                            if wvp is not None:
                                vmm_one(wvp, ts, hq)
                            gt_mm(ghs, ts)
                    for j in range(4):
                        OP("dve", lambda e, j=j: e.tensor_tensor(out=wT3[:, j, :], in0=GT[j].f(), in1=gel43[:, j, :],
                                                                 op=ALU.mult), [GT[j], gel4.sub(j * 256, 256)], [wT])
                    pst_["v"] = wv
                step([(0, 0, ("v", Vd[eg * 512:(eg + 1) * 512, :].rearrange("(j p) d -> p j d", p=128)))], peer)

            def drain(_w):
                wvp = pst_["v"]
                for ts in range(4):
                    for db in range(4):
                        vmm_one(wvp, ts, db)
                pst_.clear()
            step([], drain)

            def ph7(_w):
                gfb = B.sub(0, 2048)
                DMA("sp", gfb.f(), gfb_d, "gfb", writes=[gfb])
                ss = misc.sub(0, 1)
                t1 = misc.sub(1, 1)
                rstd = misc.sub(2, 1)
                junk = B.sub(2048, 1024)
                for ts in range(4):
                    hsub = hres.sub(ts * 2048, 2048)
                    OP("act", lambda e, hsub=hsub: e.activation(out=junk.b(), in_=hsub.f(), func=AF.Square,
                                                                accum_out=ss.f()), [hsub], [junk, ss])
                    OP("dve", lambda e: e.tensor_scalar(out=t1.f(), in0=ss.f(), scalar1=1.0 / D, scalar2=EPS,
                                                        op0=ALU.mult, op1=ALU.add), [ss], [t1])
                    OP("act", lambda e: e.activation(out=t1.f(), in_=t1.f(), func=AF.Sqrt), [t1], [t1])
                    OP("dve", lambda e: e.reciprocal(out=rstd.f(), in_=t1.f()), [t1], [rstd])
                    OP("dve", lambda e, hsub=hsub: e.scalar_tensor_tensor(
                        out=hsub.f(), in0=hsub.f(), scalar=rstd.f(), in1=gfb.f(), op0=ALU.mult, op1=ALU.mult),
                        [hsub, rstd, gfb], [hsub])
                    r0 = (ti - NPRE) * TT + ts * 128
                    DMA("sp", out[r0:r0 + 128, :], hsub.f(), "out", reads=[hsub])
            step([], ph7)

        for ti in range(NT):
            tile_prog(ti)
        if dbg:
            pass
        run_steps()
        S.final_wait_all("sp")
        S.emit(es)
    return nc


def prep_inputs(inp, NPRE, NMAIN, CPB, ncores, NEG_=32):
    x = np.asarray(inp["x"], np.float32)
    meta = np.asarray(inp["meta_tokens"], np.float32)
    MAIN = NMAIN * TT
    PRE = NPRE * TT
    assert PRE >= 16 + (CPB - 1) * MAIN

    def cols(v):
        return np.ascontiguousarray(np.asarray(v, np.float32).reshape(16, 128).T)
    vecs = np.concatenate([cols(inp["norm1_g"][0]), cols(inp["norm2_g"][0]), cols(inp["conv_b"][0]),
                           cols(inp["conv_ln_g"][0]), cols(inp["conv_ln_b"][0]), cols(inp["gla_norm_g"][0])], axis=1)
    convw = np.ascontiguousarray(np.asarray(inp["conv_w"][0], np.float32).T.reshape(16, 128, 31).transpose(1, 0, 2)).reshape(128, 496)
    wa2e = np.concatenate([np.asarray(inp["w_alpha2"][0], np.float32), np.asarray(inp["b_alpha"][0], np.float32)[None]], 0)
    gfb = np.ascontiguousarray(np.broadcast_to(np.asarray(inp["normf_g"], np.float32)[None], (128, D)))
    g1b = np.ascontiguousarray(np.broadcast_to(np.asarray(inp["norm1_g"][0], np.float32)[None], (128, D)))
    g2b = np.ascontiguousarray(np.broadcast_to(np.asarray(inp["norm2_g"][0], np.float32)[None], (128, D)))
    s = np.arange(128)[:, None]
    t = np.arange(128)[None, :]
    ident = (s == t).astype(np.float32)
    triinc = np.where(s <= t, -1.0 / 16.0, 0.0).astype(np.float32)
    trirev = np.where(s > t, -1.0 / 16.0, 0.0).astype(np.float32)
    masku = (s <= t).astype(np.float32)
    cst = np.concatenate([ident, triinc, trirev, masku], axis=1)
    k1 = np.asarray(inp["peer_k1"][0], np.float32).transpose(2, 0, 1).reshape(128, 1024)
    k2 = np.asarray(inp["peer_k2"][0], np.float32).transpose(2, 0, 1).reshape(128, 1024)
    k12 = np.ascontiguousarray(np.concatenate([k1, k2], axis=1))
    NE = NEG_ * 512
    UT = np.ascontiguousarray(np.asarray(inp["peer_u"][0], np.float32)[:NE].T)
    Vd = np.ascontiguousarray(np.asarray(inp["peer_v"][0], np.float32)[:NE])
    shared = dict(w_in=np.ascontiguousarray(inp["w_in"][0], dtype=np.float32), wa2e=wa2e, convw=convw, vecs=vecs, gfb=gfb, g1b=g1b, g2b=g2b, cst=cst,
                  w_co=np.ascontiguousarray(inp["w_conv_out"][0], dtype=np.float32),
                  w_go=np.ascontiguousarray(inp["w_gla_out"][0], dtype=np.float32),
                  w_o=np.ascontiguousarray(inp["w_out"][0], dtype=np.float32),
                  w_q=np.ascontiguousarray(inp["peer_wq"][0], dtype=np.float32), k12=k12, UT=UT, Vd=Vd)
    maps = []
    for c in range(ncores):
        b, q = c // CPB, c % CPB
        xin = np.zeros((PRE + MAIN, D), np.float32)
        real = 16 + q * MAIN
        xin[PRE - real:PRE - real + 16] = meta
        if q:
            xin[PRE - q * MAIN:PRE] = x[b, 0:q * MAIN]
        xin[PRE:] = x[b, q * MAIN:(q + 1) * MAIN]
        m = dict(shared)
        m["xin"] = xin
        maps.append(m)
    return maps


def kernel(**inputs):
    NPRE, NMAIN, CPB, NC = 13, 4, 4, 8
    nc = build(NPRE, NMAIN)
    maps = prep_inputs(inputs, NPRE, NMAIN, CPB, NC)
    res = run_bass_kernel_spmd(nc, maps, core_ids=list(range(NC)))
    B_ = inputs["x"].shape[0]
    outs = [res.results[c]["out"] for c in range(NC)]
    full = np.stack([np.concatenate(outs[b * CPB:(b + 1) * CPB], axis=0) for b in range(B_)], axis=0)
    return full.astype(np.float32)
```

```python
import numpy as np
from contextlib import ExitStack
import concourse.bass as bass
import concourse.mybir as mybir
from concourse.bass_utils import run_bass_kernel_spmd

F32 = mybir.dt.float32
BF16 = mybir.dt.bfloat16
AF = mybir.ActivationFunctionType
ALU = mybir.AluOpType
AX = mybir.AxisListType

D = 2048
NCH = 16
TT = 512
Q0, K0, V0, R0, A0, M0, M1, COLT = 4096, 5120, 6144, 8192, 10240, 10256, 12304, 14352
EPS = 1e-6
NEG = -1.0e30


class Sched:
    COMPUTE = ("pe", "act", "dve", "pool")
    ENGS = ("pe", "act", "dve", "pool", "sp")

    def __init__(self, nc):
        self.nc = nc
        self.ins = {e: [] for e in self.ENGS}
        self.wr = {}
        self.rd = {}
        self.seen = {e: {} for e in self.ENGS}
        self.dma_cnt = {}
        self.signal = {e: set() for e in self.COMPUTE}

    def _need(self, eng, deps):
        waits = []
        seen = self.seen[eng]
        for s, c in deps.items():
            if seen.get(s, 0) >= c:
                continue
            seen[s] = c
            waits.append((s, c))
            if s in self.COMPUTE:
                self.signal[s].add(c)
        return waits

    def op(self, eng, fn, reads=(), writes=(), dma=None):
        idx = len(self.ins[eng]) + 1
        if dma is not None:
            cnt = self.dma_cnt.get(dma, 0) + 1
            self.dma_cnt[dma] = cnt
            stream, count = ("dma", dma), cnt
        else:
            stream, count = eng, idx
        deps = {}

        def add(s, c):
            if deps.get(s, 0) < c:
                deps[s] = c
        rk = [k for b in reads for k in b.keys]
        wk = [k for b in writes for k in b.keys]
        for k in rk:
            for s, c in self.wr.get(k, {}).items():
                if s == stream and eng == "pe" and dma is None:
                    continue
                add(s, c)
        for k in wk:
            for s, c in self.rd.get(k, {}).items():
                if s == stream:
                    continue
                add(s, c)
            for s, c in self.wr.get(k, {}).items():
                if s == stream:
                    continue
                add(s, c)
        waits = self._need(eng, deps)
        for k in rk:
            d = self.rd.setdefault(k, {})
            if d.get(stream, 0) < count:
                d[stream] = count
        for k in wk:
            self.wr[k] = {stream: count}
            self.rd[k] = {}
        self.ins[eng].append(dict(fn=fn, waits=waits, dma=dma, idx=idx))

    def final_wait_all(self, eng="sp"):
        deps = {("dma", k): c for k, c in self.dma_cnt.items()}
        waits = self._need(eng, deps)
        self.ins[eng].append(dict(fn=None, waits=waits, dma=None, idx=len(self.ins[eng]) + 1))

    def emit(self, es):
        nc = self.nc
        sems = {}
        for e in self.COMPUTE:
            sems[e] = es.enter_context(nc.semaphore("s_" + e))
        for k in self.dma_cnt:
            sems[("dma", k)] = es.enter_context(nc.semaphore("d_" + str(k)))
        rank = {}
        for e in self.COMPUTE:
            rank[e] = {c: n + 1 for n, c in enumerate(sorted(self.signal[e]))}
        block = es.enter_context(nc.Block())
        engobj = {"pe": "tensor", "act": "scalar", "dve": "vector", "pool": "gpsimd", "sp": "sync"}

        def make(e):
            def body(eng):
                for rec in self.ins[e]:
                    for s, c in rec["waits"]:
                        if s in self.COMPUTE:
                            eng.wait_ge(sems[s], rank[s][c])
                        else:
                            eng.wait_ge(sems[s], 16 * c)
                    if rec["fn"] is None:
                        continue
                    inst = rec["fn"](eng)
                    if rec["dma"] is not None:
                        inst.then_inc(sems[("dma", rec["dma"])], 16)
                    elif rec["idx"] in self.signal[e]:
                        inst.then_inc(sems[e], 1)
            return body
        for e in self.ENGS:
            if self.ins[e]:
                getattr(block, engobj[e])(make(e))


class Buf:
    def __init__(self, base_ap, name, off, words, gran=128):
        self.base = base_ap
        self.off = off
        self.words = words
        self.name = name
        self.keys = [(name, s) for s in range(off // gran, (off + words + gran - 1) // gran)]

    def f(self, o=0, n=None):
        n = self.words - o if n is None else n
        assert o + n <= self.words
        return self.base[:, self.off + o:self.off + o + n]

    def b(self, o=0, n=None):
        n = self.words * 2 - o if n is None else n
        assert o + n <= self.words * 2
        return self.base[:, self.off:self.off + self.words].bitcast(BF16)[:, o:o + n]

    def sub(self, o, n):
        return Buf(self.base, self.name, self.off + o, n)


def build(NPRE, NMAIN, NEG_=32, dbg=None):
    nc = bass.Bass("TRN2", target_bir_lowering=False)
    NT = NPRE + NMAIN
    ROWS = NT * TT
    NE = NEG_ * 512

    def din(name, shape):
        return nc.dram_tensor(name, shape, F32, kind="ExternalInput").ap()
    xin = din("xin", [ROWS, D])
    w_in = din("w_in", [D, COLT])
    wa2e_d = din("wa2e", [17, 1024])
    convw_d = din("convw", [128, 16 * 31])
    vecs_d = din("vecs", [128, 6 * 16])
    gfb_d = din("gfb", [128, D])
    g1b_d = din("g1b", [128, D])
    g2b_d = din("g2b", [128, D])
    cst_d = din("cst", [128, 4 * 128])
    w_co = din("w_co", [D, D])
    w_go = din("w_go", [D, D])
    w_o = din("w_o", [D, D])
    w_q = din("w_q", [D, D])
    k12_d = din("k12", [128, 2 * 8 * 128])
    UT = din("UT", [D, NE])
    Vd = din("Vd", [NE, D])
    out = nc.dram_tensor("out", [NMAIN * TT, D], F32, kind="ExternalOutput").ap()
    dbg_out = None
    if dbg:
        dbg_out = nc.dram_tensor("dbg", [128, dbg], F32, kind="ExternalOutput").ap()

    es = ExitStack()
    with es:
        S = Sched(nc)

        def sbt(name, words):
            t = es.enter_context(nc.sbuf_tensor("sb_" + name, [128, words], F32))
            return Buf(t[:], name, 0, words)

        def pst(name):
            t = es.enter_context(nc.psum_tensor(name, [128, 512], F32))
            return Buf(t[:], name, 0, 512, gran=512)

        xnT = sbt("xnT", 4096)
        WR = [sbt("wr%d" % i, 4096) for i in range(4)]
        S32 = sbt("S32", 4096)
        halo = sbt("halo", 512)
        cst = sbt("cst", 512)
        vecs = sbt("vecs", 96)
        convw = sbt("convw", 496)
        wa2e = sbt("wa2e", 1024)
        wa16 = sbt("wa16", 128)
        misc = sbt("misc", 1024)
        identb = sbt("identb", 64)
        A = sbt("A", 8192)
        B = sbt("B", 8192)
        C = sbt("C", 8192)
        P = [pst("P%d" % i) for i in range(8)]

        ident = cst.sub(0, 128)
        triinc = cst.sub(128, 128)
        trirev = cst.sub(256, 128)
        masku = cst.sub(384, 128)
        ones_b = sbt("ones", 128)

        xnT3 = xnT.b().rearrange("p (k t) -> p k t", k=16)
        XN = [(xnT, xnT3), (C.sub(0, 4096), C.sub(0, 4096).b().rearrange("p (k t) -> p k t", k=16))]

        def vcol(i, k):
            return vecs.f(i * 16 + k, 1)
        VN1, VN2, VCB, VLG, VLB, VGG = range(6)


        accn = [0]

        def acc():
            accn[0] += 1
            return P[accn[0] % 4]

        def DMA(eng, out_ap, in_ap, key, reads=(), writes=()):
            S.op(eng, lambda e: e.dma_start(out=out_ap, in_=in_ap), reads=reads, writes=writes, dma=key)

        def OP(eng, fn, reads, writes):
            S.op(eng, fn, reads=reads, writes=writes)

        DMA("sp", cst.f(), cst_d, "cst", writes=[cst])
        DMA("sp", vecs.f(), vecs_d, "vecs", writes=[vecs])
        DMA("sp", convw.f(), convw_d, "convw", writes=[convw])
        DMA("sp", wa2e.f()[0:17, :], wa2e_d, "wa2e", writes=[wa2e])
        DMA("pool", wa16.b().rearrange("p (k c) -> p k c", k=16),
            w_in.rearrange("(k p) n -> p k n", p=128)[:, :, A0:A0 + 16], "wa16", writes=[wa16])
        OP("dve", lambda e: e.memset(S32.f(), 0.0), [], [S32])
        OP("dve", lambda e: e.memset(halo.f(), 0.0), [], [halo])
        OP("dve", lambda e: e.memset(ones_b.f(), 1.0), [], [ones_b])
        OP("dve", lambda e: e.tensor_copy(out=identb.b(), in_=ident.f()), [ident], [identb])

        steps = []

        def wgroup(mat, c0, n=512):
            return mat.rearrange("(k p) n -> p k n", p=128)[:, :, c0:c0 + n]

        def run_steps():
            LOOK = 2
            nload = [0]

            def issue(i):
                loads, _ = steps[i]
                buf = WR[i % 4]
                for (c_off, ncol, src) in loads:
                    if src is None:
                        continue
                    dst = buf.b().rearrange("p (k c) -> p k c", k=16)[:, :, c_off:c_off + ncol] if src[0] == "w" else \
                        buf.b().rearrange("p (j d) -> p j d", j=4)
                    DMA("pool", dst, src[1], "wr%d" % (i % 4), writes=[buf])
            for i in range(min(LOOK, len(steps))):
                issue(i)
            for i in range(len(steps)):
                steps[i][1](WR[i % 4])
                if i + LOOK < len(steps):
                    issue(i + LOOK)

        def step(loads, fn):
            steps.append((loads, fn))

        def w3(buf):
            return buf.b().rearrange("p (k c) -> p k c", k=16)

        def norm_batch(srcs, gb, xs2, junk, xn, groups=((0, 1, 2, 3),), loader=None):
            xb, x3 = xn
            ss4 = misc.sub(768, 4)
            t4 = misc.sub(896, 4)
            r4 = misc.sub(0, 4)
            for grp in groups:
                g0, gn = grp[0], len(grp)
                for ts in grp:
                    if loader is not None:
                        loader(ts)
                    OP("act", lambda e, ts=ts: e.activation(out=junk.b(), in_=srcs[ts].f(), func=AF.Square,
                                                            accum_out=ss4.f(ts, 1)), [srcs[ts]], [junk, ss4])
                OP("dve", lambda e, g0=g0, gn=gn: e.tensor_scalar(out=t4.f(g0, gn), in0=ss4.f(g0, gn), scalar1=1.0 / D,
                                                                  scalar2=EPS, op0=ALU.mult, op1=ALU.add), [ss4], [t4])
                OP("act", lambda e, g0=g0, gn=gn: e.activation(out=t4.f(g0, gn), in_=t4.f(g0, gn), func=AF.Sqrt),
                   [t4], [t4])
                OP("dve", lambda e, g0=g0, gn=gn: e.reciprocal(out=r4.f(g0, gn), in_=t4.f(g0, gn)), [t4], [r4])
                for ts in grp:
                    xs = xs2[ts % 2]
                    OP("dve", lambda e, ts=ts, xs=xs: e.scalar_tensor_tensor(
                        out=xs.b(), in0=srcs[ts].f(), scalar=r4.f(ts, 1), in1=gb.f(), op0=ALU.mult, op1=ALU.mult),
                        [srcs[ts], r4, gb], [xs])
                    for half in range(2):
                        pt = P[4 + half]

                        def fnt(e, half=half, pt=pt, xs=xs):
                            last = None
                            for k8 in range(8):
                                k = half * 8 + k8
                                last = e.transpose(out=pt.b()[:, k8 * 128:(k8 + 1) * 128],
                                                   in_=xs.b()[:, k * 128:(k + 1) * 128], identity=identb.b())
                            return last
                        OP("pe", fnt, [xs, identb], [pt])
                        OP("act", lambda e, half=half, pt=pt, ts=ts: e.copy(
                            out=x3[:, half * 8:half * 8 + 8, ts * 128:(ts + 1) * 128],
                            in_=pt.b().rearrange("p (k t) -> p k t", k=8)), [pt], [xb])

        def proj_fm(wbuf, c0, dst_ps, xn=None):
            xb, x3 = xn if xn is not None else (xnT, xnT3)

            def fn(e):
                last = None
                for k in range(16):
                    last = e.matmul(dst_ps.f(), lhsT=w3(wbuf)[:, k, c0:c0 + 128], rhs=x3[:, k, :],
                                    start=(k == 0), stop=(k == 15))
                return last
            OP("pe", fn, [wbuf, xb], [dst_ps])

        def proj_tm(wbuf, c0, n, ts, dst_ps, xn=None):
            xb, x3 = xn if xn is not None else (xnT, xnT3)

            def fn(e):
                last = None
                for k in range(16):
                    last = e.matmul(dst_ps.f(0, n), lhsT=x3[:, k, ts * 128:(ts + 1) * 128],
                                    rhs=w3(wbuf)[:, k, c0:c0 + n], start=(k == 0), stop=(k == 15))
                return last
            OP("pe", fn, [wbuf, xb], [dst_ps])

        def tile_prog(ti):
            main = ti >= NPRE
            lastpre = ti == NPRE - 1
            row0 = ti * TT
            XNt = XN[0] if main else XN[ti % 2]

            def emit_ph1(tj):
                mainj = tj >= NPRE
                rowj = tj * TT
                xnj = XN[0] if mainj else XN[tj % 2]

                def ph1(_w):
                    if mainj:
                        srcs = [A.sub(ts * 2048, 2048) for ts in range(4)]
                        gb = C.sub(0, 2048)
                        xs2_ = [B.sub(2048, 1024), B.sub(3072, 1024)]
                        junk_ = B.sub(4096, 1024)
                        groups = ((0, 1, 2, 3),)
                        keys = ["xst0", "xst1", "xst2", "xst3"]
                    else:
                        stage = [C.sub(4096, 2048), C.sub(6144, 2048)]
                        srcs = [stage[ts % 2] for ts in range(4)]
                        gb = A.sub(1024, 2048)
                        xs2_ = [A.sub(3072, 1024), A.sub(4096, 1024)]
                        junk_ = A.sub(5120, 1024)
                        groups = ((0, 1), (2, 3))
                        keys = ["xst0", "xst1", "xst0", "xst1"]
                    DMA("sp", gb.f(), g1b_d, "gb", writes=[gb])

                    def loader(ts):
                        DMA("sp", srcs[ts].f(), xin[rowj + ts * 128: rowj + (ts + 1) * 128, :], keys[ts],
                            writes=[srcs[ts]])
                    norm_batch(srcs, gb, xs2_, junk_, xnj, groups=groups, loader=loader)
                step([], ph1)

            if ti == 0 or main:
                emit_ph1(ti)

            cacc = A
            cacc3 = A.f().rearrange("p (k t) -> p k t", k=16)
            sconv = C.sub(0, 4096)
            sconv3 = sconv.b().rearrange("p (k t) -> p k t", k=16)
            mixT = C.sub(4096, 4096)
            mixT3 = mixT.b().rearrange("p (k t) -> p k t", k=16)
            ofT = sconv
            ofT3 = sconv3

            if main or lastpre:
                dgb = [B.sub(0, 2048), B.sub(2048, 2048)]
                hTb = [B.sub(4096, 384), B.sub(4480, 384)]
                sigt = B.sub(4864, 512)
                sqt = B.sub(5376, 512)
                S1b, S2b = P[6], P[7]

                def build_diag(blk):
                    dg = dgb[blk % 2]
                    for w in range(31):
                        OP("dve", lambda e, dg=dg, blk=blk, w=w: e.tensor_scalar(
                            out=dg.b(w * 128, 128), in0=ident.f(), scalar1=convw.f(blk * 31 + w, 1),
                            scalar2=None, op0=ALU.mult), [ident, convw], [dg.sub(w * 64, 64)])
                for cg in range(4):
                    holder = {}

                    def keep(wa, holder=holder):
                        holder["wa"] = wa
                    step([(0, 512, ("w", wgroup(w_in, cg * 512)))], keep)

                    def conv_g(wg, cg=cg, holder=holder):
                        wa = holder["wa"]
                        for cb in range(4):
                            blk = cg * 4 + cb
                            hb = hTb[blk % 2]
                            dg = dgb[blk % 2]
                            pa, pg = acc(), acc()
                            proj_fm(wa, cb * 128, pa, XNt)
                            proj_fm(wg, cb * 128, pg, XNt)
                            OP("act", lambda e, pg=pg: e.activation(out=sigt.f(), in_=pg.f(), func=AF.Sigmoid),
                               [pg], [sigt])
                            OP("dve", lambda e, hb=hb, blk=blk: e.tensor_copy(
                                out=hb.b(0, 32), in_=halo.b(blk * 32, 32)), [halo], [hb])
                            OP("dve", lambda e, hb=hb, pa=pa: e.tensor_tensor(
                                out=hb.b(32, 512), in0=pa.f(), in1=sigt.f(), op=ALU.mult), [pa, sigt], [hb])
                            OP("dve", lambda e, hb=hb, blk=blk: e.tensor_copy(
                                out=halo.b(blk * 32, 32), in_=hb.b(512, 32)), [hb], [halo])
                            if not main:
                                continue
                            cblk = cacc.sub(blk * 512, 512)
                            if blk == 0:
                                build_diag(0)
                            pc = acc()

                            def fnc(e, pc=pc, dg=dg, hb=hb):
                                last = None
                                for w in range(31):
                                    last = e.matmul(pc.f(), lhsT=dg.b(w * 128, 128), rhs=hb.b(w + 2, 512),
                                                    start=(w == 0), stop=(w == 30))
                                return last
                            OP("pe", fnc, [dg, hb], [pc])
                            OP("act", lambda e, cblk=cblk, pc=pc, blk=blk: e.activation(
                                out=cblk.f(), in_=pc.f(), func=AF.Identity, bias=vcol(VCB, blk)), [pc, vecs], [cblk])
                            OP("act", lambda e, pc=pc, blk=blk: e.activation(
                                out=sqt.f(), in_=pc.f(), func=AF.Square, bias=vcol(VCB, blk)), [pc, vecs], [sqt])
                            OP("pe", lambda e, cblk=cblk, blk=blk: e.matmul(
                                S1b.f(), lhsT=ones_b.f(), rhs=cblk.f(), start=(blk == 0), stop=(blk == 15)),
                               [ones_b, cblk], [S1b])
                            OP("pe", lambda e, blk=blk: e.matmul(
                                S2b.f(), lhsT=ones_b.f(), rhs=sqt.f(), start=(blk == 0), stop=(blk == 15)),
                               [ones_b, sqt], [S2b])
                            if blk + 1 < 16:
                                build_diag(blk + 1)
                    step([(0, 512, ("w", wgroup(w_in, 2048 + cg * 512)))], conv_g)

            if main:
                mu = B.sub(6144, 512)
                Ar = B.sub(6656, 512)
                Bm = B.sub(7168, 512)
                zt = B.sub(7680, 512)
                gt = B.sub(6144, 512)

                def ln_apply(_w):
                    S1b, S2b = P[6], P[7]
                    OP("dve", lambda e: e.tensor_scalar(out=mu.f(), in0=S1b.f(), scalar1=1.0 / D, scalar2=None,
                                                        op0=ALU.mult), [S1b], [mu])
                    OP("dve", lambda e: e.tensor_tensor(out=zt.f(), in0=mu.f(), in1=mu.f(), op=ALU.mult), [mu], [zt])
                    OP("dve", lambda e: e.scalar_tensor_tensor(out=Ar.f(), in0=S2b.f(), scalar=1.0 / D, in1=zt.f(),
                                                               op0=ALU.mult, op1=ALU.subtract), [S2b, zt], [Ar])
                    OP("dve", lambda e: e.tensor_scalar(out=Ar.f(), in0=Ar.f(), scalar1=EPS, scalar2=None,
                                                        op0=ALU.add), [Ar], [Ar])
                    OP("act", lambda e: e.activation(out=Ar.f(), in_=Ar.f(), func=AF.Sqrt), [Ar], [Ar])
                    OP("dve", lambda e: e.reciprocal(out=Ar.f(), in_=Ar.f()), [Ar], [Ar])
                    OP("dve", lambda e: e.scalar_tensor_tensor(out=Bm.f(), in0=mu.f(), scalar=-1.0, in1=Ar.f(),
                                                               op0=ALU.mult, op1=ALU.mult), [mu, Ar], [Bm])
                    for blk in range(16):
                        cblk = cacc.sub(blk * 512, 512)
                        OP("dve", lambda e, cblk=cblk: e.tensor_tensor(out=zt.f(), in0=cblk.f(), in1=Ar.f(),
                                                                       op=ALU.mult), [cblk, Ar], [zt])
                        OP("dve", lambda e: e.tensor_tensor(out=zt.f(), in0=zt.f(), in1=Bm.f(), op=ALU.add),
                           [zt, Bm], [zt])
                        OP("act", lambda e, blk=blk: e.activation(
                            out=sconv3[:, blk, :], in_=zt.f(), func=AF.Silu, bias=vcol(VLB, blk),
                            scale=vcol(VLG, blk)), [zt, vecs], [sconv])
                step([], ln_apply)

                def outproj_gate(first, act3, actbuf, wmat, mcol0):
                    for dg in range(4):
                        holder = {}

                        def keep(w, holder=holder):
                            holder["w"] = w
                        step([(0, 512, ("w", wgroup(wmat, dg * 512)))], keep)

                        def cons(wm, dg=dg, holder=holder):
                            wo = holder["w"]
                            for db in range(4):
                                dblk = dg * 4 + db
                                py, pm = acc(), acc()

                                def fn(e, db=db, py=py):
                                    last = None
                                    for k in range(16):
                                        last = e.matmul(py.f(), lhsT=w3(wo)[:, k, db * 128:(db + 1) * 128],
                                                        rhs=act3[:, k, :], start=(k == 0), stop=(k == 15))
                                    return last
                                OP("pe", fn, [wo, actbuf], [py])
                                proj_fm(wm, db * 128, pm)
                                OP("act", lambda e, pm=pm: e.activation(out=gt.f(), in_=pm.f(), func=AF.Sigmoid),
                                   [pm], [gt])
                                if first:
                                    OP("dve", lambda e, py=py, dblk=dblk: e.tensor_tensor(
                                        out=mixT3[:, dblk, :], in0=py.f(), in1=gt.f(), op=ALU.mult),
                                        [py, gt], [mixT])
                                else:
                                    OP("dve", lambda e, py=py: e.tensor_tensor(
                                        out=zt.f(), in0=py.f(), in1=gt.f(), op=ALU.mult), [py, gt], [zt])
                                    OP("dve", lambda e, dblk=dblk: e.tensor_tensor(
                                        out=mixT3[:, dblk, :], in0=zt.f(), in1=mixT3[:, dblk, :], op=ALU.add),
                                        [zt, mixT], [mixT])
                        step([(0, 512, ("w", wgroup(w_in, mcol0 + dg * 512)))], cons)
                outproj_gate(True, sconv3, sconv, w_co, M0)

            qTh = B.sub(0, 512)
            kTh = B.sub(512, 512)
            ktok = B.sub(1024, 1024)
            vbf = B.sub(2048, 1024)
            lbuf = B.sub(3072, 1024)
            EbT = B.sub(4096, 1024)
            Erev = B.sub(5120, 1024)
            EnbT = lbuf
            aTe = A.sub(0, 512)
            khat = A.sub(512, 512)
            qtl = A.sub(1024, 512)
            ktl = A.sub(1536, 512)
            Sbf2 = [A.sub(2048, 512), A.sub(2560, 512)]
            attm = A.sub(3072, 256)
            rs4 = A.sub(3328, 512)
            ocp = A.sub(3840, 2048)
            sq4 = A.sub(5888, 2048)
            rt = sq4.sub(0, 512)
            qTh3 = qTh.b().rearrange("p (c t) -> p c t", c=2)
            kTh3 = kTh.b().rearrange("p (c t) -> p c t", c=2)
            qTh4 = qTh.b().rearrange("p (c s t) -> p s c t", c=2, s=4)
            kTh4 = kTh.b().rearrange("p (c s t) -> p s c t", c=2, s=4)
            ktok3 = ktok.f().rearrange("p (s c) -> p s c", s=4)
            vbf3 = vbf.b().rearrange("p (s c) -> p s c", s=4)
            S323 = S32.f().rearrange("p (k e) -> p k e", k=8)
            EbT4 = EbT.f().rearrange("p (s c t) -> p s c t", s=4, c=2)
            EnbT4 = EnbT.f().rearrange("p (s c t) -> p s c t", s=4, c=2)
            qtl4 = qtl.b().rearrange("p (s c t) -> p s c t", s=4, c=2)
            ktl4 = ktl.b().rearrange("p (s c t) -> p s c t", s=4, c=2)

            def gla_pre(_w):
                pa = acc()
                OP("dve", lambda e: e.memset(aTe.f()[0:32, :], 1.0), [], [aTe])

                def fn(e):
                    last = None
                    wv = wa16.b().rearrange("p (k c) -> p k c", k=16)
                    for k in range(16):
                        last = e.matmul(pa.f()[0:16, :], lhsT=wv[:, k, :], rhs=XNt[1][:, k, :],
                                        start=(k == 0), stop=(k == 15))
                    return last
                OP("pe", fn, [wa16, XNt[0]], [pa])
                OP("act", lambda e: e.copy(out=aTe.f()[0:16, :], in_=pa.f()[0:16, :]), [pa], [aTe])
            step([], gla_pre)

            for h in range(4):
                def gla_qk(wqk, h=h):
                    if main:
                        for c in range(2):
                            pq = acc()
                            proj_fm(wqk, c * 128, pq)
                            OP("act", lambda e, pq=pq, c=c: e.activation(out=qTh3[:, c, :], in_=pq.f(), func=AF.Copy,
                                                                         scale=0.0625), [pq], [qTh])
                            pk = acc()
                            proj_fm(wqk, 256 + c * 128, pk)
                            OP("act", lambda e, pk=pk, c=c: e.copy(out=kTh3[:, c, :], in_=pk.f()), [pk], [kTh])
                    PZ = [P[4], P[5]]
                    PB = [P[6], P[7]]

                    def fnz(e):
                        last = None
                        for ts in range(4):
                            last = e.matmul(PZ[ts // 2].f((ts % 2) * 256, 256),
                                            lhsT=aTe.f()[0:17, ts * 128:(ts + 1) * 128],
                                            rhs=wa2e.f()[0:17, h * 256:(h + 1) * 256], start=True, stop=True)
                        return last
                    OP("pe", fnz, [aTe, wa2e], PZ)
                    for hf in range(2):
                        OP("act", lambda e, hf=hf: e.activation(out=lbuf.f(hf * 512, 512), in_=PZ[hf].f(),
                                                                func=AF.Exp, scale=-1.0),
                           [PZ[hf]], [lbuf.sub(hf * 512, 512)])
                    OP("act", lambda e: e.activation(out=lbuf.f(), in_=lbuf.f(), func=AF.Ln, bias=1.0), [lbuf], [lbuf])

                    for ts in range(4):
                        pk = acc()
                        proj_tm(wqk, 256, 256, ts, pk, XNt)
                        OP("act", lambda e, pk=pk, ts=ts: e.copy(out=ktok3[:, ts, :], in_=pk.f(0, 256)), [pk], [ktok])
                    def fnb(e):
                        last = None
                        for ts in range(4):
                            for c in range(2):
                                last = e.matmul(PB[ts // 2].f((ts % 2) * 256 + c * 128, 128),
                                                lhsT=lbuf.f(ts * 256 + c * 128, 128), rhs=triinc.f(),
                                                start=True, stop=True)
                        return last
                    OP("pe", fnb, [lbuf, triinc], PB)

                    def fnr(e):
                        last = None
                        for ts in range(4):
                            last = e.matmul(PZ[ts // 2].f((ts % 2) * 256, 256), lhsT=trirev.f(),
                                            rhs=lbuf.f(ts * 256, 256), start=True, stop=True)
                        return last
                    OP("pe", fnr, [lbuf, trirev], PZ)
                qk_loads = [(256, 256, ("w", wgroup(w_in, K0 + h * 256, 256)))]
                if main:
                    qk_loads.insert(0, (0, 256, ("w", wgroup(w_in, Q0 + h * 256, 256))))
                step(qk_loads, gla_qk)

                def gla_v(wv, h=h):
                    def emit_vproj():
                        for ts in range(4):
                            pv = acc()
                            proj_tm(wv, 0, 512, ts, pv, XNt)
                            OP("act", lambda e, pv=pv, ts=ts: e.copy(out=vbf3[:, ts, :], in_=pv.f()), [pv], [vbf])
                    PZ = [P[4], P[5]]
                    PB = [P[6], P[7]]
                    for hf in range(2):
                        OP("act", lambda e, hf=hf: e.activation(out=EbT.f(hf * 512, 512), in_=PB[hf].f(), func=AF.Exp),
                           [PB[hf]], [EbT.sub(hf * 512, 512)])
                        OP("act", lambda e, hf=hf: e.activation(out=Erev.f(hf * 512, 512), in_=PZ[hf].f(), func=AF.Exp),
                           [PZ[hf]], [Erev.sub(hf * 512, 512)])
                    OP("dve", lambda e: e.tensor_tensor(out=khat.b(), in0=ktok.f(), in1=Erev.f(), op=ALU.mult),
                       [ktok, Erev], [khat])
                    if main:
                        for hf in range(2):
                            OP("act", lambda e, hf=hf: e.activation(out=EnbT.f(hf * 512, 512), in_=PB[hf].f(),
                                                                    func=AF.Exp, scale=-1.0),
                               [PB[hf]], [EnbT.sub(hf * 512, 512)])
                        OP("dve", lambda e: e.tensor_tensor(out=qtl4, in0=qTh4, in1=EbT4, op=ALU.mult),
                           [qTh, EbT], [qtl])
                        OP("dve", lambda e: e.tensor_tensor(out=ktl4, in0=kTh4, in1=EnbT4, op=ALU.mult),
                           [kTh, EnbT], [ktl])
                        emit_vproj()
                        pat = acc()

                        def fna(e, pat=pat):
                            last = None
                            for ts in range(4):
                                for c in range(2):
                                    last = e.matmul(pat.f(ts * 128, 128), lhsT=ktl4[:, ts, c, :], rhs=qtl4[:, ts, c, :],
                                                    start=(c == 0), stop=(c == 1))
                            return last
                        OP("pe", fna, [ktl, qtl], [pat])
                        OP("dve", lambda e, pat=pat: e.tensor_tensor(
                            out=attm.b().rearrange("p (s i) -> p s i", s=4),
                            in0=pat.f().rearrange("p (s i) -> p s i", s=4),
                            in1=masku.f().unsqueeze(1).to_broadcast([128, 4, 128]), op=ALU.mult),
                            [pat, masku], [attm])
                    if not main:
                        emit_vproj()
                    for ts in range(4):
                        pSs = []
                        for c in range(2):
                            pS = acc()
                            OP("pe", lambda e, pS=pS, c=c, ts=ts: e.matmul(
                                pS.f(), lhsT=khat.b(ts * 256 + c * 128, 128), rhs=vbf3[:, ts, :], start=True, stop=True),
                               [khat, vbf], [pS])
                            pSs.append(pS)
                        if main:
                            Sbf = Sbf2[ts % 2]
                            Sbf3 = Sbf.b().rearrange("p (c e) -> p c e", c=2)
                            OP("act", lambda e, Sbf3=Sbf3: e.copy(out=Sbf3, in_=S323[:, 2 * h:2 * h + 2, :]),
                               [S32.sub(2 * h * 512, 1024)], [Sbf])
                            po = acc()

                            def fno(e, po=po, ts=ts, Sbf3=Sbf3):
                                last = None
                                for eb in range(4):
                                    e.matmul(po.f(eb * 128, 128), lhsT=vbf3[:, ts, eb * 128:(eb + 1) * 128],
                                             rhs=attm.b(ts * 128, 128), start=True, stop=False)
                                    for c in range(2):
                                        last = e.matmul(po.f(eb * 128, 128), lhsT=Sbf3[:, c, eb * 128:(eb + 1) * 128],
                                                        rhs=qtl4[:, ts, c, :], start=False, stop=(c == 1))
                                return last
                            OP("pe", fno, [vbf, attm, Sbf, qtl], [po])
                            OP("act", lambda e, po=po, ts=ts: e.copy(out=ocp.f(ts * 512, 512), in_=po.f()),
                               [po], [ocp.sub(ts * 512, 512)])
                            OP("act", lambda e, po=po, ts=ts: e.activation(out=sq4.f(ts * 512, 512), in_=po.f(),
                                                                          func=AF.Square),
                               [po], [sq4.sub(ts * 512, 512)])
                        for c in range(2):
                            sblk = S32.sub((2 * h + c) * 512, 512)
                            OP("dve", lambda e, pS=pSs[c], c=c, ts=ts: e.scalar_tensor_tensor(
                                out=S323[:, 2 * h + c, :], in0=S323[:, 2 * h + c, :],
                                scalar=EbT.f(ts * 256 + c * 128 + 127, 1), in1=pS.f(), op0=ALU.mult, op1=ALU.add),
                               [sblk, EbT, pSs[c]], [sblk])
                    if main:
                        pss = acc()

                        def fns(e, pss=pss):
                            last = None
                            for ts in range(4):
                                for eb in range(4):
                                    last = e.matmul(pss.f(ts * 128, 128), lhsT=ones_b.f(),
                                                    rhs=sq4.f(ts * 512 + eb * 128, 128), start=(eb == 0), stop=(eb == 3))
                            return last
                        OP("pe", fns, [ones_b, sq4], [pss])
                        OP("dve", lambda e, pss=pss: e.tensor_scalar(out=rs4.f(), in0=pss.f(), scalar1=1.0 / 512,
                                                                     scalar2=EPS, op0=ALU.mult, op1=ALU.add),
                           [pss], [rs4])
                        OP("act", lambda e: e.activation(out=rs4.f(), in_=rs4.f(), func=AF.Sqrt), [rs4], [rs4])
                        OP("dve", lambda e: e.reciprocal(out=rs4.f(), in_=rs4.f()), [rs4], [rs4])
                        OP("dve", lambda e: e.tensor_tensor(
                            out=ofT3[:, 4 * h:4 * h + 4, :].rearrange("p e (s i) -> p s e i", s=4),
                            in0=ocp.f().rearrange("p (s e i) -> p s e i", s=4, e=4),
                            in1=rs4.f().rearrange("p (s i) -> p s i", s=4).unsqueeze(2).to_broadcast([128, 4, 4, 128]),
                            op=ALU.mult), [ocp, rs4], [ofT])
                step([(0, 512, ("w", wgroup(w_in, V0 + h * 512)))], gla_v)
                if (not main) and h == 1 and ti + 1 < NPRE:
                    emit_ph1(ti + 1)

                if main:
                    def gla_r(wr_, h=h):
                        for eb in range(4):
                            pr = acc()
                            proj_fm(wr_, eb * 128, pr)
                            OP("act", lambda e, pr=pr: e.activation(out=rt.f(), in_=pr.f(), func=AF.Silu), [pr], [rt])
                            OP("dve", lambda e, eb=eb: e.scalar_tensor_tensor(
                                out=ofT3[:, 4 * h + eb, :], in0=rt.f(), scalar=vcol(VGG, 4 * h + eb),
                                in1=ofT3[:, 4 * h + eb, :], op0=ALU.mult, op1=ALU.mult), [rt, vecs, ofT], [ofT])
                    step([(0, 512, ("w", wgroup(w_in, R0 + h * 512)))], gla_r)

            if not main:
                return
            zt = B.sub(7680, 512)
            gt = B.sub(6144, 512)
            outproj_gate(False, ofT3, ofT, w_go, M1)

            hres = A
            hres3 = A.f().rearrange("p (s d) -> p s d", s=4)
            for dg in range(4):
                def oproj(wo, dg=dg):
                    if dg == 0:
                        for ts in range(4):
                            DMA("sp", hres3[:, ts, :], xin[row0 + ts * 128: row0 + (ts + 1) * 128, :], "hres%d" % ts,
                                writes=[hres.sub(ts * 2048, 2048)])
                    for ts in range(4):
                        ph = acc()

                        def fn(e, ph=ph, ts=ts):
                            last = None
                            for k in range(16):
                                last = e.matmul(ph.f(), lhsT=mixT3[:, k, ts * 128:(ts + 1) * 128], rhs=w3(wo)[:, k, :],
                                                start=(k == 0), stop=(k == 15))
                            return last
                        OP("pe", fn, [mixT, wo], [ph])
                        hb = hres.sub(ts * 2048 + dg * 512, 512)
                        OP("dve", lambda e, ph=ph, hb=hb: e.tensor_tensor(out=hb.f(), in0=hb.f(), in1=ph.f(),
                                                                          op=ALU.add), [hb, ph], [hb])
                step([(0, 512, ("w", wgroup(w_o, dg * 512)))], oproj)

            s1b = B.sub(0, 4096)
            s2b = B.sub(4096, 4096)
            s14 = s1b.f().rearrange("p (s h n) -> p s h n", s=4, h=8)
            s24 = s2b.f().rearrange("p (s h n) -> p s h n", s=4, h=8)
            Dg = C.sub(0, 2048)
            Dg4 = Dg.b().rearrange("p (s h n) -> p s h n", s=4, h=8)
            k1T = C.sub(2048, 1024)
            k2T = C.sub(3072, 1024)
            qf = C.sub(4096, 2048)
            qf3 = qf.f().rearrange("p (b t) -> p b t", b=4)
            Pc = C.sub(6144, 2048)
            xs2 = C.sub(4096, 1024)
            T1 = misc.sub(16, 128)
            T2 = misc.sub(144, 128)
            PT = misc.sub(272, 128)
            m1 = misc.sub(400, 8)
            m2 = misc.sub(408, 8)
            Zs = misc.sub(416, 8)
            rZ = misc.sub(424, 8)
            kap = misc.sub(432, 32)
            m8 = misc.sub(464, 8)
            tmpr = misc.sub(512, 256)
            T13 = T1.f().rearrange("p (h k) -> p h k", h=8)
            T23 = T2.f().rearrange("p (h k) -> p h k", h=8)
            PT3 = PT.f().rearrange("p (h k) -> p h k", h=8)

            def ph5a(_w):
                gb2 = C.sub(0, 2048)
                DMA("sp", gb2.f(), g2b_d, "gb", writes=[gb2])
                norm_batch([hres.sub(ts * 2048, 2048) for ts in range(4)], gb2,
                           [C.sub(4096, 1024), C.sub(5120, 1024)], B.sub(4096, 1024), XN[0])
                DMA("sp", C.f(2048, 2048), k12_d, "k12", writes=[k1T, k2T])
            step([], ph5a)
            for hp in range(4):
                def ph5q(wq, hp=hp):
                    for blk in range(4):
                        pq = acc()
                        proj_fm(wq, blk * 128, pq)
                        OP("act", lambda e, pq=pq, blk=blk: e.copy(out=qf3[:, blk, :], in_=pq.f()), [pq], [qf])
                    for hh in range(2):
                        h = 2 * hp + hh
                        for ts in range(4):
                            p1, p2 = P[4 + (ts % 2) * 2], P[5 + (ts % 2) * 2]
                            OP("pe", lambda e, p1=p1, hh=hh, h=h, ts=ts: e.matmul(
                                p1.f(0, 128), lhsT=qf3[:, 2 * hh, ts * 128:(ts + 1) * 128], rhs=k1T.f(h * 128, 128),
                                start=True, stop=True), [qf, k1T], [p1])
                            OP("pe", lambda e, p2=p2, hh=hh, h=h, ts=ts: e.matmul(
                                p2.f(0, 128), lhsT=qf3[:, 2 * hh + 1, ts * 128:(ts + 1) * 128], rhs=k2T.f(h * 128, 128),
                                start=True, stop=True), [qf, k2T], [p2])
                            OP("act", lambda e, p1=p1, h=h, ts=ts: e.copy(out=s14[:, ts, h, :], in_=p1.f(0, 128)),
                               [p1], [s1b.sub(ts * 1024 + h * 128, 128)])
                            OP("act", lambda e, p2=p2, h=h, ts=ts: e.copy(out=s24[:, ts, h, :], in_=p2.f(0, 128)),
                               [p2], [s2b.sub(ts * 1024 + h * 128, 128)])
                step([(0, 512, ("w", wgroup(w_q, hp * 512)))], ph5q)

            ACT_HEADS = (6, 7)
            POOL_GH_HEADS = ()

            def top16(src_ap_fn, srcbuf, dst3, dstbuf, h, nsrc):
                OP("dve", lambda e: e.max(out=dst3[:, h, 0:8], in_=src_ap_fn()), [srcbuf], [dstbuf])
                OP("dve", lambda e: e.match_replace(out=tmpr.f(0, nsrc), in_to_replace=dst3[:, h, 0:8],
                                                    in_values=src_ap_fn(), imm_value=NEG), [srcbuf, dstbuf], [tmpr])
                OP("dve", lambda e: e.max(out=dst3[:, h, 8:16], in_=tmpr.f(0, nsrc)), [tmpr], [dstbuf])

            def ph5t(_w):
                for ts in range(4):
                    s1t = s1b.sub(ts * 1024, 1024)
                    s2t = s2b.sub(ts * 1024, 1024)
                    for h in range(8):
                        top16(lambda h=h, ts=ts: s14[:, ts, h, :], s1t, T13, T1, h, 128)
                        top16(lambda h=h, ts=ts: s24[:, ts, h, :], s2t, T23, T2, h, 128)
                    OP("dve", lambda e: e.tensor_copy(out=m1.f(), in_=T13[:, :, 0]), [T1], [m1])
                    OP("dve", lambda e: e.tensor_copy(out=m2.f(), in_=T23[:, :, 0]), [T2], [m2])
                    for (sb_, s4, mm, Tb, T3) in ((s1t, s14, m1, T1, T13), (s2t, s24, m2, T2, T23)):
                        OP("dve", lambda e, s4=s4, mm=mm, ts=ts: e.tensor_tensor(
                            out=s4[:, ts, :, :], in0=s4[:, ts, :, :],
                            in1=mm.f().unsqueeze(2).to_broadcast([128, 8, 128]), op=ALU.subtract), [sb_, mm], [sb_])
                        OP("act", lambda e, s4=s4, ts=ts: e.activation(out=s4[:, ts, :, :], in_=s4[:, ts, :, :],
                                                                       func=AF.Exp), [sb_], [sb_])
                        OP("dve", lambda e, T3=T3, mm=mm: e.tensor_tensor(
                            out=T3, in0=T3, in1=mm.f().unsqueeze(2).to_broadcast([128, 8, 16]), op=ALU.subtract),
                            [Tb, mm], [Tb])
                        OP("act", lambda e, T3=T3: e.activation(out=T3, in_=T3, func=AF.Exp), [Tb], [Tb])
                    Pc4 = Pc.f().rearrange("p (h a b) -> p h a b", h=8, a=16)
                    OP("dve", lambda e: e.tensor_tensor(
                        out=Pc4, in0=T13.unsqueeze(3).to_broadcast([128, 8, 16, 16]),
                        in1=T23.unsqueeze(2).to_broadcast([128, 8, 16, 16]), op=ALU.mult), [T1, T2], [Pc])
                    for h in range(8):
                        top16(lambda h=h: Pc.f(h * 256, 256), Pc, PT3, PT, h, 256)
                    OP("dve", lambda e: e.tensor_reduce(out=Zs.f(), in_=PT3, axis=AX.X, op=ALU.add), [PT], [Zs])
                    OP("dve", lambda e: e.reciprocal(out=rZ.f(), in_=Zs.f()), [Zs], [rZ])
                    OP("dve", lambda e, ts=ts: e.tensor_copy(out=kap.f(ts * 8, 8), in_=PT3[:, :, 15]), [PT], [kap])
                    for h in range(8):
                        OP("dve", lambda e, ts=ts, h=h: e.tensor_scalar(
                            out=Dg4[:, ts, h, :], in0=ident.f(), scalar1=rZ.f(h, 1), scalar2=None, op0=ALU.mult),
                            [ident, rZ], [Dg])
            step([], ph5t)

            LP = 4096
            Pb = [C.sub(LP + i * 512, 512) for i in range(3)]
            Gh = [C.sub(LP + 1536 + i * 256, 256) for i in range(2)]
            gel4 = C.sub(LP + 2048, 1024)
            gel43 = gel4.b().rearrange("p (j t) -> p j t", j=4)
            wT = C.sub(LP + 3072, 1024)
            wT3 = wT.b().rearrange("p (j t) -> p j t", j=4)
            HT = P[0]
            YB = [P[1], P[6], P[7]]
            GT = [P[2], P[3], P[4], P[5]]
            cnt = [0, 0, 0]
            pst_ = {}

            def vmm_one(wvp, ts, db):
                cnt[2] += 1
                py = YB[cnt[2] % 3]
                wv3 = wvp.b().rearrange("p (j d) -> p j d", j=4)

                def fnv(e, py=py, ts=ts, db=db):
                    last = None
                    for j in range(4):
                        last = e.matmul(py.f(), lhsT=wT3[:, j, ts * 128:(ts + 1) * 128],
                                        rhs=wv3[:, j, db * 512:(db + 1) * 512], start=(j == 0), stop=(j == 3))
                    return last
                OP("pe", fnv, [wT, wvp], [py])
                hb = hres.sub(ts * 2048 + db * 512, 512)
                OP("dve", lambda e, py=py, hb=hb: e.tensor_tensor(out=hb.f(), in0=hb.f(), in1=py.f(),
                                                                  op=ALU.add), [hb, py], [hb])

            def gbuild_pair(eg, ts, hq):
                ghs = []
                for h in (2 * hq, 2 * hq + 1):
                    cnt[0] += 1
                    cnt[1] += 1
                    pb_, gh = Pb[cnt[0] % 3], Gh[cnt[1] % 2]
                    if h in ACT_HEADS:
                        for a in range(4):
                            OP("act", lambda e, pb_=pb_, ts=ts, h=h, a=a: e.activation(
                                out=pb_.f(a * 128, 128), in_=s24[:, ts, h, :], func=AF.Copy,
                                scale=s14[:, ts, h, 4 * eg + a:4 * eg + a + 1]),
                                [s1b.sub(ts * 1024 + h * 128, 128), s2b.sub(ts * 1024 + h * 128, 128)],
                                [pb_.sub(a * 128, 128)])
                    else:
                        OP("pool", lambda e, pb_=pb_, ts=ts, h=h: e.tensor_tensor(
                            out=pb_.f().rearrange("p (a n) -> p a n", a=4),
                            in0=s14[:, ts, h, 4 * eg:4 * eg + 4].unsqueeze(2).to_broadcast([128, 4, 128]),
                            in1=s24[:, ts, h, :].unsqueeze(1).to_broadcast([128, 4, 128]), op=ALU.mult),
                            [s1b.sub(ts * 1024 + h * 128, 128), s2b.sub(ts * 1024 + h * 128, 128)], [pb_])
                    OP("pool" if h in POOL_GH_HEADS else "dve", lambda e, pb_=pb_, gh=gh, ts=ts, h=h: e.scalar_tensor_tensor(
                        out=gh.b(), in0=pb_.f(), scalar=kap.f(ts * 8 + h, 1), in1=pb_.f(),
                        op0=ALU.is_ge, op1=ALU.mult), [pb_, kap], [gh])
                    ghs.append((gh, h))
                return ghs

            def gt_mm(ghs, ts):
                for gh, h in ghs:
                    def fng(e, gh=gh, ts=ts, h=h):
                        last = None
                        for j in range(4):
                            last = e.matmul(GT[j].f(ts * 128, 128), lhsT=gh.b(j * 128, 128),
                                            rhs=Dg4[:, ts, h, :], start=(h == 0), stop=(h == 7))
                        return last
                    OP("pe", fng, [gh, Dg], GT)

            for eg in range(NEG_):
                holder = {}

                def keepu(w, holder=holder):
                    holder["u"] = w
                step([(0, 512, ("w", wgroup(UT, eg * 512)))], keepu)

                def peer(wv, eg=eg, holder=holder):
                    wu = holder["u"]
                    wvp = pst_.get("v")
                    for ts in range(4):
                        proj_fm(wu, ts * 128, HT)
                        OP("act", lambda e, ts=ts: e.activation(out=gel43[:, ts, :], in_=HT.f(), func=AF.Gelu),
                           [HT], [gel4.sub(ts * 256, 256)])
                        for hq in range(4):
                            ghs = gbuild_pair(eg, ts, hq)
                            if wvp is not None:
                                vmm_one(wvp, ts, hq)
                            gt_mm(ghs, ts)
                    for j in range(4):
                        OP("dve", lambda e, j=j: e.tensor_tensor(out=wT3[:, j, :], in0=GT[j].f(), in1=gel43[:, j, :],
                                                                 op=ALU.mult), [GT[j], gel4.sub(j * 256, 256)], [wT])
                    pst_["v"] = wv
                step([(0, 0, ("v", Vd[eg * 512:(eg + 1) * 512, :].rearrange("(j p) d -> p j d", p=128)))], peer)

            def drain(_w):
                wvp = pst_["v"]
                for ts in range(4):
                    for db in range(4):
                        vmm_one(wvp, ts, db)
                pst_.clear()
            step([], drain)

            def ph7(_w):
                gfb = B.sub(0, 2048)
                DMA("sp", gfb.f(), gfb_d, "gfb", writes=[gfb])
                ss = misc.sub(0, 1)
                t1 = misc.sub(1, 1)
                rstd = misc.sub(2, 1)
                junk = B.sub(2048, 1024)
                for ts in range(4):
                    hsub = hres.sub(ts * 2048, 2048)
                    OP("act", lambda e, hsub=hsub: e.activation(out=junk.b(), in_=hsub.f(), func=AF.Square,
                                                                accum_out=ss.f()), [hsub], [junk, ss])
                    OP("dve", lambda e: e.tensor_scalar(out=t1.f(), in0=ss.f(), scalar1=1.0 / D, scalar2=EPS,
                                                        op0=ALU.mult, op1=ALU.add), [ss], [t1])
                    OP("act", lambda e: e.activation(out=t1.f(), in_=t1.f(), func=AF.Sqrt), [t1], [t1])
                    OP("dve", lambda e: e.reciprocal(out=rstd.f(), in_=t1.f()), [t1], [rstd])
                    OP("dve", lambda e, hsub=hsub: e.scalar_tensor_tensor(
                        out=hsub.f(), in0=hsub.f(), scalar=rstd.f(), in1=gfb.f(), op0=ALU.mult, op1=ALU.mult),
                        [hsub, rstd, gfb], [hsub])
                    r0 = (ti - NPRE) * TT + ts * 128
                    DMA("sp", out[r0:r0 + 128, :], hsub.f(), "out", reads=[hsub])
            step([], ph7)

        for ti in range(NT):
            tile_prog(ti)
        if dbg:
            pass
        run_steps()
        S.final_wait_all("sp")
        S.emit(es)
    return nc


def prep_inputs(inp, NPRE, NMAIN, CPB, ncores, NEG_=32):
    x = np.asarray(inp["x"], np.float32)
    meta = np.asarray(inp["meta_tokens"], np.float32)
    MAIN = NMAIN * TT
    PRE = NPRE * TT
    assert PRE >= 16 + (CPB - 1) * MAIN

    def cols(v):
        return np.ascontiguousarray(np.asarray(v, np.float32).reshape(16, 128).T)
    vecs = np.concatenate([cols(inp["norm1_g"][0]), cols(inp["norm2_g"][0]), cols(inp["conv_b"][0]),
                           cols(inp["conv_ln_g"][0]), cols(inp["conv_ln_b"][0]), cols(inp["gla_norm_g"][0])], axis=1)
    convw = np.ascontiguousarray(np.asarray(inp["conv_w"][0], np.float32).T.reshape(16, 128, 31).transpose(1, 0, 2)).reshape(128, 496)
    wa2e = np.concatenate([np.asarray(inp["w_alpha2"][0], np.float32), np.asarray(inp["b_alpha"][0], np.float32)[None]], 0)
    gfb = np.ascontiguousarray(np.broadcast_to(np.asarray(inp["normf_g"], np.float32)[None], (128, D)))
    g1b = np.ascontiguousarray(np.broadcast_to(np.asarray(inp["norm1_g"][0], np.float32)[None], (128, D)))
    g2b = np.ascontiguousarray(np.broadcast_to(np.asarray(inp["norm2_g"][0], np.float32)[None], (128, D)))
    s = np.arange(128)[:, None]
    t = np.arange(128)[None, :]
    ident = (s == t).astype(np.float32)
    triinc = np.where(s <= t, -1.0 / 16.0, 0.0).astype(np.float32)
    trirev = np.where(s > t, -1.0 / 16.0, 0.0).astype(np.float32)
    masku = (s <= t).astype(np.float32)
    cst = np.concatenate([ident, triinc, trirev, masku], axis=1)
    k1 = np.asarray(inp["peer_k1"][0], np.float32).transpose(2, 0, 1).reshape(128, 1024)
    k2 = np.asarray(inp["peer_k2"][0], np.float32).transpose(2, 0, 1).reshape(128, 1024)
    k12 = np.ascontiguousarray(np.concatenate([k1, k2], axis=1))
    NE = NEG_ * 512
    UT = np.ascontiguousarray(np.asarray(inp["peer_u"][0], np.float32)[:NE].T)
    Vd = np.ascontiguousarray(np.asarray(inp["peer_v"][0], np.float32)[:NE])
    shared = dict(w_in=np.ascontiguousarray(inp["w_in"][0], dtype=np.float32), wa2e=wa2e, convw=convw, vecs=vecs, gfb=gfb, g1b=g1b, g2b=g2b, cst=cst,
                  w_co=np.ascontiguousarray(inp["w_conv_out"][0], dtype=np.float32),
                  w_go=np.ascontiguousarray(inp["w_gla_out"][0], dtype=np.float32),
                  w_o=np.ascontiguousarray(inp["w_out"][0], dtype=np.float32),
                  w_q=np.ascontiguousarray(inp["peer_wq"][0], dtype=np.float32), k12=k12, UT=UT, Vd=Vd)
    maps = []
    for c in range(ncores):
        b, q = c // CPB, c % CPB
        xin = np.zeros((PRE + MAIN, D), np.float32)
        real = 16 + q * MAIN
        xin[PRE - real:PRE - real + 16] = meta
        if q:
            xin[PRE - q * MAIN:PRE] = x[b, 0:q * MAIN]
        xin[PRE:] = x[b, q * MAIN:(q + 1) * MAIN]
        m = dict(shared)
        m["xin"] = xin
        maps.append(m)
    return maps


def kernel(**inputs):
    NPRE, NMAIN, CPB, NC = 13, 4, 4, 8
    nc = build(NPRE, NMAIN)
    maps = prep_inputs(inputs, NPRE, NMAIN, CPB, NC)
    res = run_bass_kernel_spmd(nc, maps, core_ids=list(range(NC)))
    B_ = inputs["x"].shape[0]
    outs = [res.results[c]["out"] for c in range(NC)]
    full = np.stack([np.concatenate(outs[b * CPB:(b + 1) * CPB], axis=0) for b in range(B_)], axis=0)
    return full.astype(np.float32)
```

```python
import numpy as np
from contextlib import ExitStack
import concourse.bass as bass
import concourse.mybir as mybir
from concourse.bass_utils import run_bass_kernel_spmd

F32 = mybir.dt.float32
BF16 = mybir.dt.bfloat16
AF = mybir.ActivationFunctionType
ALU = mybir.AluOpType
AX = mybir.AxisListType

D = 2048
NCH = 16
TT = 512
Q0, K0, V0, R0, A0, M0, M1, COLT = 4096, 5120, 6144, 8192, 10240, 10256, 12304, 14352
EPS = 1e-6
NEG = -1.0e30


class Sched:
    COMPUTE = ("pe", "act", "dve", "pool")
    ENGS = ("pe", "act", "dve", "pool", "sp")

    def __init__(self, nc):
        self.nc = nc
        self.ins = {e: [] for e in self.ENGS}
        self.wr = {}
        self.rd = {}
        self.seen = {e: {} for e in self.ENGS}
        self.dma_cnt = {}
        self.signal = {e: set() for e in self.COMPUTE}

    def _need(self, eng, deps):
        waits = []
        seen = self.seen[eng]
        for s, c in deps.items():
            if seen.get(s, 0) >= c:
                continue
            seen[s] = c
            waits.append((s, c))
            if s in self.COMPUTE:
                self.signal[s].add(c)
        return waits

    def op(self, eng, fn, reads=(), writes=(), dma=None):
        idx = len(self.ins[eng]) + 1
        if dma is not None:
            cnt = self.dma_cnt.get(dma, 0) + 1
            self.dma_cnt[dma] = cnt
            stream, count = ("dma", dma), cnt
        else:
            stream, count = eng, idx
        deps = {}

        def add(s, c):
            if deps.get(s, 0) < c:
                deps[s] = c
        rk = [k for b in reads for k in b.keys]
        wk = [k for b in writes for k in b.keys]
        for k in rk:
            for s, c in self.wr.get(k, {}).items():
                if s == stream and eng == "pe" and dma is None:
                    continue
                add(s, c)
        for k in wk:
            for s, c in self.rd.get(k, {}).items():
                if s == stream:
                    continue
                add(s, c)
            for s, c in self.wr.get(k, {}).items():
                if s == stream:
                    continue
                add(s, c)
        waits = self._need(eng, deps)
        for k in rk:
            d = self.rd.setdefault(k, {})
            if d.get(stream, 0) < count:
                d[stream] = count
        for k in wk:
            self.wr[k] = {stream: count}
            self.rd[k] = {}
        self.ins[eng].append(dict(fn=fn, waits=waits, dma=dma, idx=idx))

    def final_wait_all(self, eng="sp"):
        deps = {("dma", k): c for k, c in self.dma_cnt.items()}
        waits = self._need(eng, deps)
        self.ins[eng].append(dict(fn=None, waits=waits, dma=None, idx=len(self.ins[eng]) + 1))

    def emit(self, es):
        nc = self.nc
        sems = {}
        for e in self.COMPUTE:
            sems[e] = es.enter_context(nc.semaphore("s_" + e))
        for k in self.dma_cnt:
            sems[("dma", k)] = es.enter_context(nc.semaphore("d_" + str(k)))
        rank = {}
        for e in self.COMPUTE:
            rank[e] = {c: n + 1 for n, c in enumerate(sorted(self.signal[e]))}
        block = es.enter_context(nc.Block())
        engobj = {"pe": "tensor", "act": "scalar", "dve": "vector", "pool": "gpsimd", "sp": "sync"}

        def make(e):
            def body(eng):
                for rec in self.ins[e]:
                    for s, c in rec["waits"]:
                        if s in self.COMPUTE:
                            eng.wait_ge(sems[s], rank[s][c])
                        else:
                            eng.wait_ge(sems[s], 16 * c)
                    if rec["fn"] is None:
                        continue
                    inst = rec["fn"](eng)
                    if rec["dma"] is not None:
                        inst.then_inc(sems[("dma", rec["dma"])], 16)
                    elif rec["idx"] in self.signal[e]:
                        inst.then_inc(sems[e], 1)
            return body
        for e in self.ENGS:
            if self.ins[e]:
                getattr(block, engobj[e])(make(e))


class Buf:
    def __init__(self, base_ap, name, off, words, gran=128):
        self.base = base_ap
        self.off = off
        self.words = words
        self.name = name
        self.keys = [(name, s) for s in range(off // gran, (off + words + gran - 1) // gran)]

    def f(self, o=0, n=None):
        n = self.words - o if n is None else n
        assert o + n <= self.words
        return self.base[:, self.off + o:self.off + o + n]

    def b(self, o=0, n=None):
        n = self.words * 2 - o if n is None else n
        assert o + n <= self.words * 2
        return self.base[:, self.off:self.off + self.words].bitcast(BF16)[:, o:o + n]

    def sub(self, o, n):
        return Buf(self.base, self.name, self.off + o, n)


def build(NPRE, NMAIN, NEG_=32, dbg=None):
    nc = bass.Bass("TRN2", target_bir_lowering=False)
    NT = NPRE + NMAIN
    ROWS = NT * TT
    NE = NEG_ * 512

    def din(name, shape):
        return nc.dram_tensor(name, shape, F32, kind="ExternalInput").ap()
    xin = din("xin", [ROWS, D])
    w_in = din("w_in", [D, COLT])
    wa2e_d = din("wa2e", [17, 1024])
    convw_d = din("convw", [128, 16 * 31])
    vecs_d = din("vecs", [128, 6 * 16])
    gfb_d = din("gfb", [128, D])
    g1b_d = din("g1b", [128, D])
    g2b_d = din("g2b", [128, D])
    cst_d = din("cst", [128, 4 * 128])
    w_co = din("w_co", [D, D])
    w_go = din("w_go", [D, D])
    w_o = din("w_o", [D, D])
    w_q = din("w_q", [D, D])
    k12_d = din("k12", [128, 2 * 8 * 128])
    UT = din("UT", [D, NE])
    Vd = din("Vd", [NE, D])
    out = nc.dram_tensor("out", [NMAIN * TT, D], F32, kind="ExternalOutput").ap()
    dbg_out = None
    if dbg:
        dbg_out = nc.dram_tensor("dbg", [128, dbg], F32, kind="ExternalOutput").ap()

    es = ExitStack()
    with es:
        S = Sched(nc)

        def sbt(name, words):
            t = es.enter_context(nc.sbuf_tensor("sb_" + name, [128, words], F32))
            return Buf(t[:], name, 0, words)

        def pst(name):
            t = es.enter_context(nc.psum_tensor(name, [128, 512], F32))
            return Buf(t[:], name, 0, 512, gran=512)

        xnT = sbt("xnT", 4096)
        WR = [sbt("wr%d" % i, 4096) for i in range(4)]
        S32 = sbt("S32", 4096)
        halo = sbt("halo", 512)
        cst = sbt("cst", 512)
        vecs = sbt("vecs", 96)
        convw = sbt("convw", 496)
        wa2e = sbt("wa2e", 1024)
        wa16 = sbt("wa16", 128)
        misc = sbt("misc", 1024)
        identb = sbt("identb", 64)
        A = sbt("A", 8192)
        B = sbt("B", 8192)
        C = sbt("C", 8192)
        P = [pst("P%d" % i) for i in range(8)]

        ident = cst.sub(0, 128)
        triinc = cst.sub(128, 128)
        trirev = cst.sub(256, 128)
        masku = cst.sub(384, 128)
        ones_b = sbt("ones", 128)

        xnT3 = xnT.b().rearrange("p (k t) -> p k t", k=16)
        XN = [(xnT, xnT3), (C.sub(0, 4096), C.sub(0, 4096).b().rearrange("p (k t) -> p k t", k=16))]

        def vcol(i, k):
            return vecs.f(i * 16 + k, 1)
        VN1, VN2, VCB, VLG, VLB, VGG = range(6)


        accn = [0]

        def acc():
            accn[0] += 1
            return P[accn[0] % 4]

        def DMA(eng, out_ap, in_ap, key, reads=(), writes=()):
            S.op(eng, lambda e: e.dma_start(out=out_ap, in_=in_ap), reads=reads, writes=writes, dma=key)

        def OP(eng, fn, reads, writes):
            S.op(eng, fn, reads=reads, writes=writes)

        DMA("sp", cst.f(), cst_d, "cst", writes=[cst])
        DMA("sp", vecs.f(), vecs_d, "vecs", writes=[vecs])
        DMA("sp", convw.f(), convw_d, "convw", writes=[convw])
        DMA("sp", wa2e.f()[0:17, :], wa2e_d, "wa2e", writes=[wa2e])
        DMA("pool", wa16.b().rearrange("p (k c) -> p k c", k=16),
            w_in.rearrange("(k p) n -> p k n", p=128)[:, :, A0:A0 + 16], "wa16", writes=[wa16])
        OP("dve", lambda e: e.memset(S32.f(), 0.0), [], [S32])
        OP("dve", lambda e: e.memset(halo.f(), 0.0), [], [halo])
        OP("dve", lambda e: e.memset(ones_b.f(), 1.0), [], [ones_b])
        OP("dve", lambda e: e.tensor_copy(out=identb.b(), in_=ident.f()), [ident], [identb])

        steps = []

        def wgroup(mat, c0, n=512):
            return mat.rearrange("(k p) n -> p k n", p=128)[:, :, c0:c0 + n]

        def run_steps():
            LOOK = 2
            nload = [0]

            def issue(i):
                loads, _ = steps[i]
                buf = WR[i % 4]
                for (c_off, ncol, src) in loads:
                    if src is None:
                        continue
                    dst = buf.b().rearrange("p (k c) -> p k c", k=16)[:, :, c_off:c_off + ncol] if src[0] == "w" else \
                        buf.b().rearrange("p (j d) -> p j d", j=4)
                    DMA("pool", dst, src[1], "wr%d" % (i % 4), writes=[buf])
            for i in range(min(LOOK, len(steps))):
                issue(i)
            for i in range(len(steps)):
                steps[i][1](WR[i % 4])
                if i + LOOK < len(steps):
                    issue(i + LOOK)

        def step(loads, fn):
            steps.append((loads, fn))

        def w3(buf):
            return buf.b().rearrange("p (k c) -> p k c", k=16)

        def norm_batch(srcs, gb, xs2, junk, xn, groups=((0, 1, 2, 3),), loader=None):
            xb, x3 = xn
            ss4 = misc.sub(768, 4)
            t4 = misc.sub(896, 4)
            r4 = misc.sub(0, 4)
            for grp in groups:
                g0, gn = grp[0], len(grp)
                for ts in grp:
                    if loader is not None:
                        loader(ts)
                    OP("act", lambda e, ts=ts: e.activation(out=junk.b(), in_=srcs[ts].f(), func=AF.Square,
                                                            accum_out=ss4.f(ts, 1)), [srcs[ts]], [junk, ss4])
                OP("dve", lambda e, g0=g0, gn=gn: e.tensor_scalar(out=t4.f(g0, gn), in0=ss4.f(g0, gn), scalar1=1.0 / D,
                                                                  scalar2=EPS, op0=ALU.mult, op1=ALU.add), [ss4], [t4])
                OP("act", lambda e, g0=g0, gn=gn: e.activation(out=t4.f(g0, gn), in_=t4.f(g0, gn), func=AF.Sqrt),
                   [t4], [t4])
                OP("dve", lambda e, g0=g0, gn=gn: e.reciprocal(out=r4.f(g0, gn), in_=t4.f(g0, gn)), [t4], [r4])
                for ts in grp:
                    xs = xs2[ts % 2]
                    OP("dve", lambda e, ts=ts, xs=xs: e.scalar_tensor_tensor(
                        out=xs.b(), in0=srcs[ts].f(), scalar=r4.f(ts, 1), in1=gb.f(), op0=ALU.mult, op1=ALU.mult),
                        [srcs[ts], r4, gb], [xs])
                    for half in range(2):
                        pt = P[4 + half]

                        def fnt(e, half=half, pt=pt, xs=xs):
                            last = None
                            for k8 in range(8):
                                k = half * 8 + k8
                                last = e.transpose(out=pt.b()[:, k8 * 128:(k8 + 1) * 128],
                                                   in_=xs.b()[:, k * 128:(k + 1) * 128], identity=identb.b())
                            return last
                        OP("pe", fnt, [xs, identb], [pt])
                        OP("act", lambda e, half=half, pt=pt, ts=ts: e.copy(
                            out=x3[:, half * 8:half * 8 + 8, ts * 128:(ts + 1) * 128],
                            in_=pt.b().rearrange("p (k t) -> p k t", k=8)), [pt], [xb])

        def norm_pieces(srcs, gb, xs2, junk, xn, loader):
            xb, x3 = xn
            ss4 = misc.sub(768, 4)
            t4 = misc.sub(896, 4)
            r4 = misc.sub(0, 4)
            pieces = []
            for grp in ((0, 1), (2, 3)):
                def pre(grp=grp):
                    g0, gn = grp[0], 2
                    for ts in grp:
                        loader(ts)
                        OP("act", lambda e, ts=ts: e.activation(out=junk.b(), in_=srcs[ts].f(), func=AF.Square,
                                                                accum_out=ss4.f(ts, 1)), [srcs[ts]], [junk, ss4])
                    OP("dve", lambda e: e.tensor_scalar(out=t4.f(g0, gn), in0=ss4.f(g0, gn), scalar1=1.0 / D,
                                                        scalar2=EPS, op0=ALU.mult, op1=ALU.add), [ss4], [t4])
                    OP("act", lambda e: e.activation(out=t4.f(g0, gn), in_=t4.f(g0, gn), func=AF.Sqrt), [t4], [t4])
                    OP("dve", lambda e: e.reciprocal(out=r4.f(g0, gn), in_=t4.f(g0, gn)), [t4], [r4])
                    for ts in grp:
                        xs = xs2[ts % 2]
                        OP("dve", lambda e, ts=ts, xs=xs: e.scalar_tensor_tensor(
                            out=xs.b(), in0=srcs[ts].f(), scalar=r4.f(ts, 1), in1=gb.f(), op0=ALU.mult, op1=ALU.mult),
                            [srcs[ts], r4, gb], [xs])

                def tr(grp=grp):
                    for ts in grp:
                        xs = xs2[ts % 2]
                        for half in range(2):
                            pt = P[4 + half]

                            def fnt(e, half=half, pt=pt, xs=xs):
                                last = None
                                for k8 in range(8):
                                    k = half * 8 + k8
                                    last = e.transpose(out=pt.b()[:, k8 * 128:(k8 + 1) * 128],
                                                       in_=xs.b()[:, k * 128:(k + 1) * 128], identity=identb.b())
                                return last
                            OP("pe", fnt, [xs, identb], [pt])
                            OP("act", lambda e, half=half, pt=pt, ts=ts: e.copy(
                                out=x3[:, half * 8:half * 8 + 8, ts * 128:(ts + 1) * 128],
                                in_=pt.b().rearrange("p (k t) -> p k t", k=8)), [pt], [xb])
                pieces += [pre, tr]
            return pieces

        def proj_fm(wbuf, c0, dst_ps, xn=None):
            xb, x3 = xn if xn is not None else (xnT, xnT3)

            def fn(e):
                last = None
                for k in range(16):
                    last = e.matmul(dst_ps.f(), lhsT=w3(wbuf)[:, k, c0:c0 + 128], rhs=x3[:, k, :],
                                    start=(k == 0), stop=(k == 15))
                return last
            OP("pe", fn, [wbuf, xb], [dst_ps])

        def proj_tm(wbuf, c0, n, ts, dst_ps, xn=None):
            xb, x3 = xn if xn is not None else (xnT, xnT3)

            def fn(e):
                last = None
                for k in range(16):
                    last = e.matmul(dst_ps.f(0, n), lhsT=x3[:, k, ts * 128:(ts + 1) * 128],
                                    rhs=w3(wbuf)[:, k, c0:c0 + n], start=(k == 0), stop=(k == 15))
                return last
            OP("pe", fn, [wbuf, xb], [dst_ps])

        def tile_prog(ti):
            main = ti >= NPRE
            lastpre = ti == NPRE - 1
            row0 = ti * TT
            XNt = XN[0] if main else XN[ti % 2]

            def emit_ph1(tj, split=False):
                mainj = tj >= NPRE
                rowj = tj * TT
                xnj = XN[0] if mainj else XN[tj % 2]

                def ph1(_w):
                    if mainj:
                        srcs = [A.sub(ts * 2048, 2048) for ts in range(4)]
                        gb = C.sub(0, 2048)
                        xs2_ = [B.sub(2048, 1024), B.sub(3072, 1024)]
                        junk_ = B.sub(4096, 1024)
                        groups = ((0, 1, 2, 3),)
                        keys = ["xst0", "xst1", "xst2", "xst3"]
                    else:
                        stage = [C.sub(4096, 2048), C.sub(6144, 2048)]
                        srcs = [stage[ts % 2] for ts in range(4)]
                        gb = A.sub(1024, 2048)
                        xs2_ = [A.sub(3072, 1024), A.sub(4096, 1024)]
                        junk_ = A.sub(5120, 1024)
                        groups = ((0, 1), (2, 3))
                        keys = ["xst0", "xst1", "xst0", "xst1"]
                    def loader(ts):
                        DMA("sp", srcs[ts].f(), xin[rowj + ts * 128: rowj + (ts + 1) * 128, :], keys[ts],
                            writes=[srcs[ts]])
                    if mainj:
                        DMA("sp", gb.f(), g1b_d, "gb", writes=[gb])
                        norm_batch(srcs, gb, xs2_, junk_, xnj, groups=groups, loader=loader)
                        return None
                    pcs = norm_pieces(srcs, gb, xs2_, junk_, xnj, loader)
                    pre0 = pcs[0]

                    def pre0g():
                        DMA("sp", gb.f(), g1b_d, "gb", writes=[gb])
                        pre0()
                    return [pre0g, pcs[1], pcs[2], pcs[3]]
                if split:
                    holder = {}

                    def s0(_w):
                        holder["p"] = ph1(None)
                        holder["p"][0]()

                    def s1(_w):
                        holder["p"][1]()
                        holder["p"][2]()

                    def s2(_w):
                        holder["p"][3]()
                    return [s0, s1, s2]

                def allp(_w):
                    p = ph1(None)
                    if p is not None:
                        for f in p:
                            f()
                step([], allp)
                return None

            if ti == 0 or main:
                emit_ph1(ti)

            cacc = A
            cacc3 = A.f().rearrange("p (k t) -> p k t", k=16)
            sconv = C.sub(0, 4096)
            sconv3 = sconv.b().rearrange("p (k t) -> p k t", k=16)
            mixT = C.sub(4096, 4096)
            mixT3 = mixT.b().rearrange("p (k t) -> p k t", k=16)
            ofT = sconv
            ofT3 = sconv3

            if main or lastpre:
                dgb = [B.sub(0, 2048), B.sub(2048, 2048)]
                hTb = [B.sub(4096, 384), B.sub(4480, 384)]
                sigt = B.sub(4864, 512)
                sqt = B.sub(5376, 512)
                S1b, S2b = P[6], P[7]

                def build_diag(blk):
                    dg = dgb[blk % 2]
                    for w in range(31):
                        OP("dve", lambda e, dg=dg, blk=blk, w=w: e.tensor_scalar(
                            out=dg.b(w * 128, 128), in0=ident.f(), scalar1=convw.f(blk * 31 + w, 1),
                            scalar2=None, op0=ALU.mult), [ident, convw], [dg.sub(w * 64, 64)])
                for cg in range(4):
                    holder = {}

                    def keep(wa, holder=holder):
                        holder["wa"] = wa
                    step([(0, 512, ("w", wgroup(w_in, cg * 512)))], keep)

                    def conv_g(wg, cg=cg, holder=holder):
                        wa = holder["wa"]
                        for cb in range(4):
                            blk = cg * 4 + cb
                            hb = hTb[blk % 2]
                            dg = dgb[blk % 2]
                            pa, pg = acc(), acc()
                            proj_fm(wa, cb * 128, pa, XNt)
                            proj_fm(wg, cb * 128, pg, XNt)
                            OP("act", lambda e, pg=pg: e.activation(out=sigt.f(), in_=pg.f(), func=AF.Sigmoid),
                               [pg], [sigt])
                            OP("dve", lambda e, hb=hb, blk=blk: e.tensor_copy(
                                out=hb.b(0, 32), in_=halo.b(blk * 32, 32)), [halo], [hb])
                            OP("dve", lambda e, hb=hb, pa=pa: e.tensor_tensor(
                                out=hb.b(32, 512), in0=pa.f(), in1=sigt.f(), op=ALU.mult), [pa, sigt], [hb])
                            OP("dve", lambda e, hb=hb, blk=blk: e.tensor_copy(
                                out=halo.b(blk * 32, 32), in_=hb.b(512, 32)), [hb], [halo])
                            if not main:
                                continue
                            cblk = cacc.sub(blk * 512, 512)
                            if blk == 0:
                                build_diag(0)
                            pc = acc()

                            def fnc(e, pc=pc, dg=dg, hb=hb):
                                last = None
                                for w in range(31):
                                    last = e.matmul(pc.f(), lhsT=dg.b(w * 128, 128), rhs=hb.b(w + 2, 512),
                                                    start=(w == 0), stop=(w == 30))
                                return last
                            OP("pe", fnc, [dg, hb], [pc])
                            OP("act", lambda e, cblk=cblk, pc=pc, blk=blk: e.activation(
                                out=cblk.f(), in_=pc.f(), func=AF.Identity, bias=vcol(VCB, blk)), [pc, vecs], [cblk])
                            OP("act", lambda e, pc=pc, blk=blk: e.activation(
                                out=sqt.f(), in_=pc.f(), func=AF.Square, bias=vcol(VCB, blk)), [pc, vecs], [sqt])
                            OP("pe", lambda e, cblk=cblk, blk=blk: e.matmul(
                                S1b.f(), lhsT=ones_b.f(), rhs=cblk.f(), start=(blk == 0), stop=(blk == 15)),
                               [ones_b, cblk], [S1b])
                            OP("pe", lambda e, blk=blk: e.matmul(
                                S2b.f(), lhsT=ones_b.f(), rhs=sqt.f(), start=(blk == 0), stop=(blk == 15)),
                               [ones_b, sqt], [S2b])
                            if blk + 1 < 16:
                                build_diag(blk + 1)
                    step([(0, 512, ("w", wgroup(w_in, 2048 + cg * 512)))], conv_g)

            if main:
                mu = B.sub(6144, 512)
                Ar = B.sub(6656, 512)
                Bm = B.sub(7168, 512)
                zt = B.sub(7680, 512)
                gt = B.sub(6144, 512)

                def ln_apply(_w):
                    S1b, S2b = P[6], P[7]
                    OP("dve", lambda e: e.tensor_scalar(out=mu.f(), in0=S1b.f(), scalar1=1.0 / D, scalar2=None,
                                                        op0=ALU.mult), [S1b], [mu])
                    OP("dve", lambda e: e.tensor_tensor(out=zt.f(), in0=mu.f(), in1=mu.f(), op=ALU.mult), [mu], [zt])
                    OP("dve", lambda e: e.scalar_tensor_tensor(out=Ar.f(), in0=S2b.f(), scalar=1.0 / D, in1=zt.f(),
                                                               op0=ALU.mult, op1=ALU.subtract), [S2b, zt], [Ar])
                    OP("dve", lambda e: e.tensor_scalar(out=Ar.f(), in0=Ar.f(), scalar1=EPS, scalar2=None,
                                                        op0=ALU.add), [Ar], [Ar])
                    OP("act", lambda e: e.activation(out=Ar.f(), in_=Ar.f(), func=AF.Sqrt), [Ar], [Ar])
                    OP("dve", lambda e: e.reciprocal(out=Ar.f(), in_=Ar.f()), [Ar], [Ar])
                    OP("dve", lambda e: e.scalar_tensor_tensor(out=Bm.f(), in0=mu.f(), scalar=-1.0, in1=Ar.f(),
                                                               op0=ALU.mult, op1=ALU.mult), [mu, Ar], [Bm])
                    for blk in range(16):
                        cblk = cacc.sub(blk * 512, 512)
                        zb = (zt, mu)[blk % 2]
                        OP("dve", lambda e, cblk=cblk, zb=zb: e.tensor_tensor(out=zb.f(), in0=cblk.f(), in1=Ar.f(),
                                                                              op=ALU.mult), [cblk, Ar], [zb])
                        OP("dve", lambda e, zb=zb: e.tensor_tensor(out=zb.f(), in0=zb.f(), in1=Bm.f(), op=ALU.add),
                           [zb, Bm], [zb])
                        OP("act", lambda e, blk=blk, zb=zb: e.activation(
                            out=sconv3[:, blk, :], in_=zb.f(), func=AF.Silu, bias=vcol(VLB, blk),
                            scale=vcol(VLG, blk)), [zb, vecs], [sconv.sub(blk * 256, 256)])
                step([], ln_apply)

                def outproj_gate(first, act3, actbuf, wmat, mcol0):
                    for dg in range(4):
                        holder = {}

                        def keep(w, holder=holder):
                            holder["w"] = w
                        step([(0, 512, ("w", wgroup(wmat, dg * 512)))], keep)

                        def cons(wm, dg=dg, holder=holder):
                            wo = holder["w"]
                            for db in range(4):
                                dblk = dg * 4 + db
                                py, pm = acc(), acc()

                                def fn(e, db=db, py=py):
                                    last = None
                                    for k in range(16):
                                        last = e.matmul(py.f(), lhsT=w3(wo)[:, k, db * 128:(db + 1) * 128],
                                                        rhs=act3[:, k, :], start=(k == 0), stop=(k == 15))
                                    return last
                                OP("pe", fn, [wo, actbuf], [py])
                                proj_fm(wm, db * 128, pm)
                                OP("act", lambda e, pm=pm: e.activation(out=gt.f(), in_=pm.f(), func=AF.Sigmoid),
                                   [pm], [gt])
                                if first:
                                    OP("dve", lambda e, py=py, dblk=dblk: e.tensor_tensor(
                                        out=mixT3[:, dblk, :], in0=py.f(), in1=gt.f(), op=ALU.mult),
                                        [py, gt], [mixT])
                                else:
                                    OP("dve", lambda e, py=py: e.tensor_tensor(
                                        out=zt.f(), in0=py.f(), in1=gt.f(), op=ALU.mult), [py, gt], [zt])
                                    OP("dve", lambda e, dblk=dblk: e.tensor_tensor(
                                        out=mixT3[:, dblk, :], in0=zt.f(), in1=mixT3[:, dblk, :], op=ALU.add),
                                        [zt, mixT], [mixT])
                        step([(0, 512, ("w", wgroup(w_in, mcol0 + dg * 512)))], cons)
                outproj_gate(True, sconv3, sconv, w_co, M0)

            qTh = B.sub(0, 512)
            kTh = B.sub(512, 512)
            ktok = B.sub(1024, 1024)
            vbf = B.sub(2048, 1024)
            lbuf = B.sub(3072, 1024)
            EbT = B.sub(4096, 1024)
            Erev = B.sub(5120, 1024)
            EnbT = lbuf
            aTe = A.sub(0, 512)
            khat = A.sub(512, 512)
            qtl = A.sub(1024, 512)
            ktl = A.sub(1536, 512)
            Sbf2 = [A.sub(2048, 512), A.sub(2560, 512)]
            attm = A.sub(3072, 256)
            rs4 = A.sub(3328, 512)
            ocp = A.sub(3840, 2048)
            sq4 = A.sub(5888, 2048)
            rt = sq4.sub(0, 512)
            qTh3 = qTh.b().rearrange("p (c t) -> p c t", c=2)
            kTh3 = kTh.b().rearrange("p (c t) -> p c t", c=2)
            qTh4 = qTh.b().rearrange("p (c s t) -> p s c t", c=2, s=4)
            kTh4 = kTh.b().rearrange("p (c s t) -> p s c t", c=2, s=4)
            ktok3 = ktok.f().rearrange("p (s c) -> p s c", s=4)
            vbf3 = vbf.b().rearrange("p (s c) -> p s c", s=4)
            S323 = S32.f().rearrange("p (k e) -> p k e", k=8)
            EbT4 = EbT.f().rearrange("p (s c t) -> p s c t", s=4, c=2)
            EnbT4 = EnbT.f().rearrange("p (s c t) -> p s c t", s=4, c=2)
            qtl4 = qtl.b().rearrange("p (s c t) -> p s c t", s=4, c=2)
            ktl4 = ktl.b().rearrange("p (s c t) -> p s c t", s=4, c=2)

            def gla_pre(_w):
                pa = acc()
                OP("dve", lambda e: e.memset(aTe.f()[0:32, :], 1.0), [], [aTe])

                def fn(e):
                    last = None
                    wv = wa16.b().rearrange("p (k c) -> p k c", k=16)
                    for k in range(16):
                        last = e.matmul(pa.f()[0:16, :], lhsT=wv[:, k, :], rhs=XNt[1][:, k, :],
                                        start=(k == 0), stop=(k == 15))
                    return last
                OP("pe", fn, [wa16, XNt[0]], [pa])
                OP("act", lambda e: e.copy(out=aTe.f()[0:16, :], in_=pa.f()[0:16, :]), [pa], [aTe])
            step([], gla_pre)

            for h in range(4):
                def gla_qk(wqk, h=h):
                    if main:
                        for c in range(2):
                            pq = acc()
                            proj_fm(wqk, c * 128, pq)
                            OP("act", lambda e, pq=pq, c=c: e.activation(out=qTh3[:, c, :], in_=pq.f(), func=AF.Copy,
                                                                         scale=0.0625), [pq], [qTh])
                            pk = acc()
                            proj_fm(wqk, 256 + c * 128, pk)
                            OP("act", lambda e, pk=pk, c=c: e.copy(out=kTh3[:, c, :], in_=pk.f()), [pk], [kTh])
                    PZ = [P[4], P[5]]
                    PB = [P[6], P[7]]

                    def fnz(e):
                        last = None
                        for ts in range(4):
                            last = e.matmul(PZ[ts // 2].f((ts % 2) * 256, 256),
                                            lhsT=aTe.f()[0:17, ts * 128:(ts + 1) * 128],
                                            rhs=wa2e.f()[0:17, h * 256:(h + 1) * 256], start=True, stop=True)
                        return last
                    OP("pe", fnz, [aTe, wa2e], PZ)
                    for hf in range(2):
                        OP("act", lambda e, hf=hf: e.activation(out=lbuf.f(hf * 512, 512), in_=PZ[hf].f(),
                                                                func=AF.Exp, scale=-1.0),
                           [PZ[hf]], [lbuf.sub(hf * 512, 512)])
                    OP("act", lambda e: e.activation(out=lbuf.f(), in_=lbuf.f(), func=AF.Ln, bias=1.0), [lbuf], [lbuf])

                    for ts in range(4):
                        pk = acc()
                        proj_tm(wqk, 256, 256, ts, pk, XNt)
                        OP("act", lambda e, pk=pk, ts=ts: e.copy(out=ktok3[:, ts, :], in_=pk.f(0, 256)), [pk], [ktok])
                    def fnb(e):
                        last = None
                        for ts in range(4):
                            for c in range(2):
                                last = e.matmul(PB[ts // 2].f((ts % 2) * 256 + c * 128, 128),
                                                lhsT=lbuf.f(ts * 256 + c * 128, 128), rhs=triinc.f(),
                                                start=True, stop=True)
                        return last
                    OP("pe", fnb, [lbuf, triinc], PB)

                    def fnr(e):
                        last = None
                        for ts in range(4):
                            last = e.matmul(PZ[ts // 2].f((ts % 2) * 256, 256), lhsT=trirev.f(),
                                            rhs=lbuf.f(ts * 256, 256), start=True, stop=True)
                        return last
                    OP("pe", fnr, [lbuf, trirev], PZ)
                qk_loads = [(256, 256, ("w", wgroup(w_in, K0 + h * 256, 256)))]
                if main:
                    qk_loads.insert(0, (0, 256, ("w", wgroup(w_in, Q0 + h * 256, 256))))
                step(qk_loads, gla_qk)

                def gla_v(wv, h=h):
                    def emit_vproj():
                        for ts in range(4):
                            pv = acc()
                            proj_tm(wv, 0, 512, ts, pv, XNt)
                            OP("act", lambda e, pv=pv, ts=ts: e.copy(out=vbf3[:, ts, :], in_=pv.f()), [pv], [vbf])
                    PZ = [P[4], P[5]]
                    PB = [P[6], P[7]]
                    for hf in range(2):
                        OP("act", lambda e, hf=hf: e.activation(out=EbT.f(hf * 512, 512), in_=PB[hf].f(), func=AF.Exp),
                           [PB[hf]], [EbT.sub(hf * 512, 512)])
                        OP("act", lambda e, hf=hf: e.activation(out=Erev.f(hf * 512, 512), in_=PZ[hf].f(), func=AF.Exp),
                           [PZ[hf]], [Erev.sub(hf * 512, 512)])
                    OP("dve", lambda e: e.tensor_tensor(out=khat.b(), in0=ktok.f(), in1=Erev.f(), op=ALU.mult),
                       [ktok, Erev], [khat])
                    if main:
                        for hf in range(2):
                            OP("act", lambda e, hf=hf: e.activation(out=EnbT.f(hf * 512, 512), in_=PB[hf].f(),
                                                                    func=AF.Exp, scale=-1.0),
                               [PB[hf]], [EnbT.sub(hf * 512, 512)])
                        OP("dve", lambda e: e.tensor_tensor(out=qtl4, in0=qTh4, in1=EbT4, op=ALU.mult),
                           [qTh, EbT], [qtl])
                        OP("dve", lambda e: e.tensor_tensor(out=ktl4, in0=kTh4, in1=EnbT4, op=ALU.mult),
                           [kTh, EnbT], [ktl])
                        emit_vproj()
                        pat = acc()

                        def fna(e, pat=pat):
                            last = None
                            for ts in range(4):
                                for c in range(2):
                                    last = e.matmul(pat.f(ts * 128, 128), lhsT=ktl4[:, ts, c, :], rhs=qtl4[:, ts, c, :],
                                                    start=(c == 0), stop=(c == 1))
                            return last
                        OP("pe", fna, [ktl, qtl], [pat])
                        OP("dve", lambda e, pat=pat: e.tensor_tensor(
                            out=attm.b().rearrange("p (s i) -> p s i", s=4),
                            in0=pat.f().rearrange("p (s i) -> p s i", s=4),
                            in1=masku.f().unsqueeze(1).to_broadcast([128, 4, 128]), op=ALU.mult),
                            [pat, masku], [attm])
                    if not main:
                        emit_vproj()
                    for ts in range(4):
                        pSs = []
                        for c in range(2):
                            pS = acc()
                            OP("pe", lambda e, pS=pS, c=c, ts=ts: e.matmul(
                                pS.f(), lhsT=khat.b(ts * 256 + c * 128, 128), rhs=vbf3[:, ts, :], start=True, stop=True),
                               [khat, vbf], [pS])
                            pSs.append(pS)
                        if main:
                            Sbf = Sbf2[ts % 2]
                            Sbf3 = Sbf.b().rearrange("p (c e) -> p c e", c=2)
                            OP("act", lambda e, Sbf3=Sbf3: e.copy(out=Sbf3, in_=S323[:, 2 * h:2 * h + 2, :]),
                               [S32.sub(2 * h * 512, 1024)], [Sbf])
                            po = acc()

                            def fno(e, po=po, ts=ts, Sbf3=Sbf3):
                                last = None
                                for eb in range(4):
                                    e.matmul(po.f(eb * 128, 128), lhsT=vbf3[:, ts, eb * 128:(eb + 1) * 128],
                                             rhs=attm.b(ts * 128, 128), start=True, stop=False)
                                    for c in range(2):
                                        last = e.matmul(po.f(eb * 128, 128), lhsT=Sbf3[:, c, eb * 128:(eb + 1) * 128],
                                                        rhs=qtl4[:, ts, c, :], start=False, stop=(c == 1))
                                return last
                            OP("pe", fno, [vbf, attm, Sbf, qtl], [po])
                            OP("act", lambda e, po=po, ts=ts: e.copy(out=ocp.f(ts * 512, 512), in_=po.f()),
                               [po], [ocp.sub(ts * 512, 512)])
                            OP("act", lambda e, po=po, ts=ts: e.activation(out=sq4.f(ts * 512, 512), in_=po.f(),
                                                                          func=AF.Square),
                               [po], [sq4.sub(ts * 512, 512)])
                        for c in range(2):
                            sblk = S32.sub((2 * h + c) * 512, 512)
                            OP("dve", lambda e, pS=pSs[c], c=c, ts=ts: e.scalar_tensor_tensor(
                                out=S323[:, 2 * h + c, :], in0=S323[:, 2 * h + c, :],
                                scalar=EbT.f(ts * 256 + c * 128 + 127, 1), in1=pS.f(), op0=ALU.mult, op1=ALU.add),
                               [sblk, EbT, pSs[c]], [sblk])
                    if main:
                        pss = acc()

                        def fns(e, pss=pss):
                            last = None
                            for ts in range(4):
                                for eb in range(4):
                                    last = e.matmul(pss.f(ts * 128, 128), lhsT=ones_b.f(),
                                                    rhs=sq4.f(ts * 512 + eb * 128, 128), start=(eb == 0), stop=(eb == 3))
                            return last
                        OP("pe", fns, [ones_b, sq4], [pss])
                        OP("dve", lambda e, pss=pss: e.tensor_scalar(out=rs4.f(), in0=pss.f(), scalar1=1.0 / 512,
                                                                     scalar2=EPS, op0=ALU.mult, op1=ALU.add),
                           [pss], [rs4])
                        OP("act", lambda e: e.activation(out=rs4.f(), in_=rs4.f(), func=AF.Sqrt), [rs4], [rs4])
                        OP("dve", lambda e: e.reciprocal(out=rs4.f(), in_=rs4.f()), [rs4], [rs4])
                        OP("dve", lambda e: e.tensor_tensor(
                            out=ofT3[:, 4 * h:4 * h + 4, :].rearrange("p e (s i) -> p s e i", s=4),
                            in0=ocp.f().rearrange("p (s e i) -> p s e i", s=4, e=4),
                            in1=rs4.f().rearrange("p (s i) -> p s i", s=4).unsqueeze(2).to_broadcast([128, 4, 4, 128]),
                            op=ALU.mult), [ocp, rs4], [ofT])
                step([(0, 512, ("w", wgroup(w_in, V0 + h * 512)))], gla_v)
                if (not main) and ti + 1 < NPRE:
                    if h == 0:
                        hoist = emit_ph1(ti + 1, split=True)
                    if h <= 2:
                        step([], hoist[h])

                if main:
                    def gla_r(wr_, h=h):
                        for eb in range(4):
                            pr = acc()
                            proj_fm(wr_, eb * 128, pr)
                            OP("act", lambda e, pr=pr: e.activation(out=rt.f(), in_=pr.f(), func=AF.Silu), [pr], [rt])
                            OP("dve", lambda e, eb=eb: e.scalar_tensor_tensor(
                                out=ofT3[:, 4 * h + eb, :], in0=rt.f(), scalar=vcol(VGG, 4 * h + eb),
                                in1=ofT3[:, 4 * h + eb, :], op0=ALU.mult, op1=ALU.mult), [rt, vecs, ofT], [ofT])
                    step([(0, 512, ("w", wgroup(w_in, R0 + h * 512)))], gla_r)

            if not main:
                return
            zt = B.sub(7680, 512)
            gt = B.sub(6144, 512)
            outproj_gate(False, ofT3, ofT, w_go, M1)

            hres = A
            hres3 = A.f().rearrange("p (s d) -> p s d", s=4)
            for dg in range(4):
                def oproj(wo, dg=dg):
                    if dg == 0:
                        for ts in range(4):
                            DMA("sp", hres3[:, ts, :], xin[row0 + ts * 128: row0 + (ts + 1) * 128, :], "hres%d" % ts,
                                writes=[hres.sub(ts * 2048, 2048)])
                    for ts in range(4):
                        ph = acc()

                        def fn(e, ph=ph, ts=ts):
                            last = None
                            for k in range(16):
                                last = e.matmul(ph.f(), lhsT=mixT3[:, k, ts * 128:(ts + 1) * 128], rhs=w3(wo)[:, k, :],
                                                start=(k == 0), stop=(k == 15))
                            return last
                        OP("pe", fn, [mixT, wo], [ph])
                        hb = hres.sub(ts * 2048 + dg * 512, 512)
                        OP("dve", lambda e, ph=ph, hb=hb: e.tensor_tensor(out=hb.f(), in0=hb.f(), in1=ph.f(),
                                                                          op=ALU.add), [hb, ph], [hb])
                step([(0, 512, ("w", wgroup(w_o, dg * 512)))], oproj)

            s1b = B.sub(0, 4096)
            s2b = B.sub(4096, 4096)
            s14 = s1b.f().rearrange("p (s h n) -> p s h n", s=4, h=8)
            s24 = s2b.f().rearrange("p (s h n) -> p s h n", s=4, h=8)
            Dg = C.sub(0, 2048)
            Dg4 = Dg.b().rearrange("p (s h n) -> p s h n", s=4, h=8)
            k1T = C.sub(2048, 1024)
            k2T = C.sub(3072, 1024)
            qf = C.sub(4096, 2048)
            qf3 = qf.f().rearrange("p (b t) -> p b t", b=4)
            Pc = C.sub(6144, 2048)
            xs2 = C.sub(4096, 1024)
            T1 = misc.sub(16, 128)
            T2 = misc.sub(144, 128)
            PT = misc.sub(272, 128)
            m1 = misc.sub(400, 8)
            m2 = misc.sub(408, 8)
            Zs = misc.sub(416, 8)
            rZ = misc.sub(424, 8)
            kap = misc.sub(432, 32)
            m8 = misc.sub(464, 8)
            tmpr = misc.sub(512, 256)
            T13 = T1.f().rearrange("p (h k) -> p h k", h=8)
            T23 = T2.f().rearrange("p (h k) -> p h k", h=8)
            PT3 = PT.f().rearrange("p (h k) -> p h k", h=8)

            def ph5a(_w):
                gb2 = C.sub(0, 2048)
                DMA("sp", gb2.f(), g2b_d, "gb", writes=[gb2])
                norm_batch([hres.sub(ts * 2048, 2048) for ts in range(4)], gb2,
                           [C.sub(4096, 1024), C.sub(5120, 1024)], B.sub(4096, 1024), XN[0])
                DMA("sp", C.f(2048, 2048), k12_d, "k12", writes=[k1T, k2T])
            step([], ph5a)
            for hp in range(4):
                def ph5q(wq, hp=hp):
                    for blk in range(4):
                        pq = acc()
                        proj_fm(wq, blk * 128, pq)
                        OP("act", lambda e, pq=pq, blk=blk: e.copy(out=qf3[:, blk, :], in_=pq.f()), [pq], [qf])
                    for hh in range(2):
                        h = 2 * hp + hh
                        for ts in range(4):
                            p1, p2 = P[4 + (ts % 2) * 2], P[5 + (ts % 2) * 2]
                            OP("pe", lambda e, p1=p1, hh=hh, h=h, ts=ts: e.matmul(
                                p1.f(0, 128), lhsT=qf3[:, 2 * hh, ts * 128:(ts + 1) * 128], rhs=k1T.f(h * 128, 128),
                                start=True, stop=True), [qf, k1T], [p1])
                            OP("pe", lambda e, p2=p2, hh=hh, h=h, ts=ts: e.matmul(
                                p2.f(0, 128), lhsT=qf3[:, 2 * hh + 1, ts * 128:(ts + 1) * 128], rhs=k2T.f(h * 128, 128),
                                start=True, stop=True), [qf, k2T], [p2])
                            OP("act", lambda e, p1=p1, h=h, ts=ts: e.copy(out=s14[:, ts, h, :], in_=p1.f(0, 128)),
                               [p1], [s1b.sub(ts * 1024 + h * 128, 128)])
                            OP("act", lambda e, p2=p2, h=h, ts=ts: e.copy(out=s24[:, ts, h, :], in_=p2.f(0, 128)),
                               [p2], [s2b.sub(ts * 1024 + h * 128, 128)])
                step([(0, 512, ("w", wgroup(w_q, hp * 512)))], ph5q)

            ACT_HEADS = (6, 7)
            POOL_GH_HEADS = ()

            def top16(src_ap_fn, srcbuf, dst3, dstbuf, h, nsrc):
                OP("dve", lambda e: e.max(out=dst3[:, h, 0:8], in_=src_ap_fn()), [srcbuf], [dstbuf])
                OP("dve", lambda e: e.match_replace(out=tmpr.f(0, nsrc), in_to_replace=dst3[:, h, 0:8],
                                                    in_values=src_ap_fn(), imm_value=NEG), [srcbuf, dstbuf], [tmpr])
                OP("dve", lambda e: e.max(out=dst3[:, h, 8:16], in_=tmpr.f(0, nsrc)), [tmpr], [dstbuf])

            def ph5t(_w):
                for ts in range(4):
                    s1t = s1b.sub(ts * 1024, 1024)
                    s2t = s2b.sub(ts * 1024, 1024)
                    for h in range(8):
                        top16(lambda h=h, ts=ts: s14[:, ts, h, :], s1t, T13, T1, h, 128)
                        top16(lambda h=h, ts=ts: s24[:, ts, h, :], s2t, T23, T2, h, 128)
                    OP("dve", lambda e: e.tensor_copy(out=m1.f(), in_=T13[:, :, 0]), [T1], [m1])
                    OP("dve", lambda e: e.tensor_copy(out=m2.f(), in_=T23[:, :, 0]), [T2], [m2])
                    for (sb_, s4, mm, Tb, T3) in ((s1t, s14, m1, T1, T13), (s2t, s24, m2, T2, T23)):
                        OP("dve", lambda e, s4=s4, mm=mm, ts=ts: e.tensor_tensor(
                            out=s4[:, ts, :, :], in0=s4[:, ts, :, :],
                            in1=mm.f().unsqueeze(2).to_broadcast([128, 8, 128]), op=ALU.subtract), [sb_, mm], [sb_])
                        OP("act", lambda e, s4=s4, ts=ts: e.activation(out=s4[:, ts, :, :], in_=s4[:, ts, :, :],
                                                                       func=AF.Exp), [sb_], [sb_])
                        OP("dve", lambda e, T3=T3, mm=mm: e.tensor_tensor(
                            out=T3, in0=T3, in1=mm.f().unsqueeze(2).to_broadcast([128, 8, 16]), op=ALU.subtract),
                            [Tb, mm], [Tb])
                        OP("act", lambda e, T3=T3: e.activation(out=T3, in_=T3, func=AF.Exp), [Tb], [Tb])
                    Pc4 = Pc.f().rearrange("p (h a b) -> p h a b", h=8, a=16)
                    OP("dve", lambda e: e.tensor_tensor(
                        out=Pc4, in0=T13.unsqueeze(3).to_broadcast([128, 8, 16, 16]),
                        in1=T23.unsqueeze(2).to_broadcast([128, 8, 16, 16]), op=ALU.mult), [T1, T2], [Pc])
                    for h in range(8):
                        top16(lambda h=h: Pc.f(h * 256, 256), Pc, PT3, PT, h, 256)
                    OP("dve", lambda e: e.tensor_reduce(out=Zs.f(), in_=PT3, axis=AX.X, op=ALU.add), [PT], [Zs])
                    OP("dve", lambda e: e.reciprocal(out=rZ.f(), in_=Zs.f()), [Zs], [rZ])
                    OP("dve", lambda e, ts=ts: e.tensor_copy(out=kap.f(ts * 8, 8), in_=PT3[:, :, 15]), [PT], [kap])
                    for h in range(8):
                        OP("dve", lambda e, ts=ts, h=h: e.tensor_scalar(
                            out=Dg4[:, ts, h, :], in0=ident.f(), scalar1=rZ.f(h, 1), scalar2=None, op0=ALU.mult),
                            [ident, rZ], [Dg])
            step([], ph5t)

            LP = 4096
            Pb = [C.sub(LP + i * 512, 512) for i in range(3)]
            Gh = [C.sub(LP + 1536 + i * 256, 256) for i in range(2)]
            gel4 = C.sub(LP + 2048, 1024)
            gel43 = gel4.b().rearrange("p (j t) -> p j t", j=4)
            wT = C.sub(LP + 3072, 1024)
            wT3 = wT.b().rearrange("p (j t) -> p j t", j=4)
            HT = P[0]
            YB = [P[1], P[6], P[7]]
            GT = [P[2], P[3], P[4], P[5]]
            cnt = [0, 0, 0]
            pst_ = {}

            def vmm_one(wvp, ts, db):
                cnt[2] += 1
                py = YB[cnt[2] % 3]
                wv3 = wvp.b().rearrange("p (j d) -> p j d", j=4)

                def fnv(e, py=py, ts=ts, db=db):
                    last = None
                    for j in range(4):
                        last = e.matmul(py.f(), lhsT=wT3[:, j, ts * 128:(ts + 1) * 128],
                                        rhs=wv3[:, j, db * 512:(db + 1) * 512], start=(j == 0), stop=(j == 3))
                    return last
                OP("pe", fnv, [wT, wvp], [py])
                hb = hres.sub(ts * 2048 + db * 512, 512)
                OP("dve", lambda e, py=py, hb=hb: e.tensor_tensor(out=hb.f(), in0=hb.f(), in1=py.f(),
                                                                  op=ALU.add), [hb, py], [hb])

            def gbuild_pair(eg, ts, hq):
                ghs = []
                for h in (2 * hq, 2 * hq + 1):
                    cnt[0] += 1
                    cnt[1] += 1
                    pb_, gh = Pb[cnt[0] % 3], Gh[cnt[1] % 2]
                    if h in ACT_HEADS:
                        for a in range(4):
                            OP("act", lambda e, pb_=pb_, ts=ts, h=h, a=a: e.activation(
                                out=pb_.f(a * 128, 128), in_=s24[:, ts, h, :], func=AF.Copy,
                                scale=s14[:, ts, h, 4 * eg + a:4 * eg + a + 1]),
                                [s1b.sub(ts * 1024 + h * 128, 128), s2b.sub(ts * 1024 + h * 128, 128)],
                                [pb_.sub(a * 128, 128)])
                    else:
                        OP("pool", lambda e, pb_=pb_, ts=ts, h=h: e.tensor_tensor(
                            out=pb_.f().rearrange("p (a n) -> p a n", a=4),
                            in0=s14[:, ts, h, 4 * eg:4 * eg + 4].unsqueeze(2).to_broadcast([128, 4, 128]),
                            in1=s24[:, ts, h, :].unsqueeze(1).to_broadcast([128, 4, 128]), op=ALU.mult),
                            [s1b.sub(ts * 1024 + h * 128, 128), s2b.sub(ts * 1024 + h * 128, 128)], [pb_])
                    OP("pool" if h in POOL_GH_HEADS else "dve", lambda e, pb_=pb_, gh=gh, ts=ts, h=h: e.scalar_tensor_tensor(
                        out=gh.b(), in0=pb_.f(), scalar=kap.f(ts * 8 + h, 1), in1=pb_.f(),
                        op0=ALU.is_ge, op1=ALU.mult), [pb_, kap], [gh])
                    ghs.append((gh, h))
                return ghs

            def gt_mm(ghs, ts):
                for gh, h in ghs:
                    def fng(e, gh=gh, ts=ts, h=h):
                        last = None
                        for j in range(4):
                            last = e.matmul(GT[j].f(ts * 128, 128), lhsT=gh.b(j * 128, 128),
                                            rhs=Dg4[:, ts, h, :], start=(h == 0), stop=(h == 7))
                        return last
                    OP("pe", fng, [gh, Dg], GT)

            for eg in range(NEG_):
                holder = {}

                def keepu(w, holder=holder):
                    holder["u"] = w
                step([(0, 512, ("w", wgroup(UT, eg * 512)))], keepu)

                def peer(wv, eg=eg, holder=holder):
                    wu = holder["u"]
                    wvp = pst_.get("v")
                    for ts in range(4):
                        proj_fm(wu, ts * 128, HT)
                        OP("act", lambda e, ts=ts: e.activation(out=gel43[:, ts, :], in_=HT.f(), func=AF.Gelu),
                           [HT], [gel4.sub(ts * 256, 256)])
                        for hq in range(4):
                            ghs = gbuild_pair(eg, ts, hq)
                            if wvp is not None:
                                vmm_one(wvp, ts, hq)
                            gt_mm(ghs, ts)
                    for j in range(4):
                        OP("dve", lambda e, j=j: e.tensor_tensor(out=wT3[:, j, :], in0=GT[j].f(), in1=gel43[:, j, :],
                                                                 op=ALU.mult), [GT[j], gel4.sub(j * 256, 256)], [wT])
                    pst_["v"] = wv
                step([(0, 0, ("v", Vd[eg * 512:(eg + 1) * 512, :].rearrange("(j p) d -> p j d", p=128)))], peer)

            def drain(_w):
                wvp = pst_["v"]
                for ts in range(4):
                    for db in range(4):
                        vmm_one(wvp, ts, db)
                pst_.clear()
            step([], drain)

            def ph7(_w):
                gfb = B.sub(0, 2048)
                DMA("sp", gfb.f(), gfb_d, "gfb", writes=[gfb])
                ss = misc.sub(0, 1)
                t1 = misc.sub(1, 1)
                rstd = misc.sub(2, 1)
                junk = B.sub(2048, 1024)
                for ts in range(4):
                    hsub = hres.sub(ts * 2048, 2048)
                    OP("act", lambda e, hsub=hsub: e.activation(out=junk.b(), in_=hsub.f(), func=AF.Square,
                                                                accum_out=ss.f()), [hsub], [junk, ss])
                    OP("dve", lambda e: e.tensor_scalar(out=t1.f(), in0=ss.f(), scalar1=1.0 / D, scalar2=EPS,
                                                        op0=ALU.mult, op1=ALU.add), [ss], [t1])
                    OP("act", lambda e: e.activation(out=t1.f(), in_=t1.f(), func=AF.Sqrt), [t1], [t1])
                    OP("dve", lambda e: e.reciprocal(out=rstd.f(), in_=t1.f()), [t1], [rstd])
                    OP("dve", lambda e, hsub=hsub: e.scalar_tensor_tensor(
                        out=hsub.f(), in0=hsub.f(), scalar=rstd.f(), in1=gfb.f(), op0=ALU.mult, op1=ALU.mult),
                        [hsub, rstd, gfb], [hsub])
                    r0 = (ti - NPRE) * TT + ts * 128
                    DMA("sp", out[r0:r0 + 128, :], hsub.f(), "out", reads=[hsub])
            step([], ph7)

        for ti in range(NT):
            tile_prog(ti)
        if dbg:
            pass
        run_steps()
        S.final_wait_all("sp")
        S.emit(es)
    return nc


def prep_inputs(inp, NPRE, NMAIN, CPB, ncores, NEG_=32):
    x = np.asarray(inp["x"], np.float32)
    meta = np.asarray(inp["meta_tokens"], np.float32)
    MAIN = NMAIN * TT
    PRE = NPRE * TT
    assert PRE >= 16 + (CPB - 1) * MAIN

    def cols(v):
        return np.ascontiguousarray(np.asarray(v, np.float32).reshape(16, 128).T)
    vecs = np.concatenate([cols(inp["norm1_g"][0]), cols(inp["norm2_g"][0]), cols(inp["conv_b"][0]),
                           cols(inp["conv_ln_g"][0]), cols(inp["conv_ln_b"][0]), cols(inp["gla_norm_g"][0])], axis=1)
    convw = np.ascontiguousarray(np.asarray(inp["conv_w"][0], np.float32).T.reshape(16, 128, 31).transpose(1, 0, 2)).reshape(128, 496)
    wa2e = np.concatenate([np.asarray(inp["w_alpha2"][0], np.float32), np.asarray(inp["b_alpha"][0], np.float32)[None]], 0)
    gfb = np.ascontiguousarray(np.broadcast_to(np.asarray(inp["normf_g"], np.float32)[None], (128, D)))
    g1b = np.ascontiguousarray(np.broadcast_to(np.asarray(inp["norm1_g"][0], np.float32)[None], (128, D)))
    g2b = np.ascontiguousarray(np.broadcast_to(np.asarray(inp["norm2_g"][0], np.float32)[None], (128, D)))
    s = np.arange(128)[:, None]
    t = np.arange(128)[None, :]
    ident = (s == t).astype(np.float32)
    triinc = np.where(s <= t, -1.0 / 16.0, 0.0).astype(np.float32)
    trirev = np.where(s > t, -1.0 / 16.0, 0.0).astype(np.float32)
    masku = (s <= t).astype(np.float32)
    cst = np.concatenate([ident, triinc, trirev, masku], axis=1)
    k1 = np.asarray(inp["peer_k1"][0], np.float32).transpose(2, 0, 1).reshape(128, 1024)
    k2 = np.asarray(inp["peer_k2"][0], np.float32).transpose(2, 0, 1).reshape(128, 1024)
    k12 = np.ascontiguousarray(np.concatenate([k1, k2], axis=1))
    NE = NEG_ * 512
    UT = np.ascontiguousarray(np.asarray(inp["peer_u"][0], np.float32)[:NE].T)
    Vd = np.ascontiguousarray(np.asarray(inp["peer_v"][0], np.float32)[:NE])
    shared = dict(w_in=np.ascontiguousarray(inp["w_in"][0], dtype=np.float32), wa2e=wa2e, convw=convw, vecs=vecs, gfb=gfb, g1b=g1b, g2b=g2b, cst=cst,
                  w_co=np.ascontiguousarray(inp["w_conv_out"][0], dtype=np.float32),
                  w_go=np.ascontiguousarray(inp["w_gla_out"][0], dtype=np.float32),
                  w_o=np.ascontiguousarray(inp["w_out"][0], dtype=np.float32),
                  w_q=np.ascontiguousarray(inp["peer_wq"][0], dtype=np.float32), k12=k12, UT=UT, Vd=Vd)
    maps = []
    for c in range(ncores):
        b, q = c // CPB, c % CPB
        xin = np.zeros((PRE + MAIN, D), np.float32)
        real = 16 + q * MAIN
        xin[PRE - real:PRE - real + 16] = meta
        if q:
            xin[PRE - q * MAIN:PRE] = x[b, 0:q * MAIN]
        xin[PRE:] = x[b, q * MAIN:(q + 1) * MAIN]
        m = dict(shared)
        m["xin"] = xin
        maps.append(m)
    return maps


def kernel(**inputs):
    NPRE, NMAIN, CPB, NC = 13, 4, 4, 8
    nc = build(NPRE, NMAIN)
    maps = prep_inputs(inputs, NPRE, NMAIN, CPB, NC)
    res = run_bass_kernel_spmd(nc, maps, core_ids=list(range(NC)))
    B_ = inputs["x"].shape[0]
    outs = [res.results[c]["out"] for c in range(NC)]
    full = np.stack([np.concatenate(outs[b * CPB:(b + 1) * CPB], axis=0) for b in range(B_)], axis=0)
    return full.astype(np.float32)
```
